# Optimizing a Trainium2 kernel written in Bass

```python
import math
import jax, jax.numpy as jnp
from jax import lax
import numpy as np

D_MODEL = 1024
BATCH = 16
SEQ = 2048
DEPTH = 1

MEM_LEN = 256
HEAD_DIM = 128
ATTN_HEADS = 6
ATTN_KV_HEADS = 2
ATTN_GROUP = ATTN_HEADS // ATTN_KV_HEADS
WINDOW = 128
BLOCK = 128
ROPE_DIM = HEAD_DIM // 4
ROPE_THETA = 500000.0
LRU_WIDTH = 768
LRU_BLOCKS = 6
LRU_BLOCK_W = LRU_WIDTH // LRU_BLOCKS
CONV_WIDTH = 4
CONV_LEFT = CONV_WIDTH // 2
LRU_C = 8.0
MEM_HEADS = 4
N_BRANCH = 3
PEER_HEADS = 8
PEER_KEYS = 128
PEER_EXPERTS = PEER_KEYS * PEER_KEYS
PEER_QDIM = 256
PEER_HALF = PEER_QDIM // 2
PEER_TOPK = 16
PEER_CHUNK = 128
EPS = 1e-6
NEG_INF = -1e30

ATTN_Q_W = ATTN_HEADS * HEAD_DIM
ATTN_KV_W = ATTN_KV_HEADS * HEAD_DIM
MEM_Q_W = MEM_HEADS * HEAD_DIM
GATE_W = N_BRANCH * D_MODEL
IN_W = ATTN_Q_W + 2 * ATTN_KV_W + 2 * LRU_WIDTH + MEM_Q_W + GATE_W

kernel_name = "hybrid_swa_rglru_mem_peer_encoder"


def rmsnorm(x, g):
    xf = x.astype(jnp.float32)
    y = xf * lax.rsqrt(jnp.mean(xf * xf, axis=-1, keepdims=True) + EPS) * g.astype(jnp.float32)
    return y.astype(x.dtype)


def partial_rope(t, positions):
    half = ROPE_DIM // 2
    inv_freq = ROPE_THETA ** (-jnp.arange(0, ROPE_DIM, 2, dtype=jnp.float32) / ROPE_DIM)
    ang = positions.astype(jnp.float32)[..., None] * inv_freq
    cos = jnp.cos(ang)[:, :, None, :]
    sin = jnp.sin(ang)[:, :, None, :]
    tr = t[..., :ROPE_DIM].astype(jnp.float32)
    t1, t2 = tr[..., :half], tr[..., half:]
    rot = jnp.concatenate([t1 * cos - t2 * sin, t2 * cos + t1 * sin], axis=-1)
    return jnp.concatenate([rot.astype(t.dtype), t[..., ROPE_DIM:]], axis=-1)


def band_blocks(t, nb):
    b = t.shape[0]
    tp = jnp.pad(t, ((0, 0), (BLOCK, BLOCK), (0, 0), (0, 0)))
    tp = tp.reshape(b, nb + 2, BLOCK, t.shape[2], t.shape[3])
    return jnp.concatenate([tp[:, :-2], tp[:, 1:-1], tp[:, 2:]], axis=2)


def windowed_gqa(q, k, v, sink):
    b, s = q.shape[0], q.shape[1]
    nb = s // BLOCK
    qb = q.reshape(b, nb, BLOCK, ATTN_KV_HEADS, ATTN_GROUP, HEAD_DIM)
    kb = band_blocks(k, nb)
    vb = band_blocks(v, nb)
    scores = jnp.einsum('bnqkgd,bnskd->bnkgqs', qb, kb).astype(jnp.float32) * (HEAD_DIM ** -0.5)
    blk = jnp.arange(nb, dtype=jnp.int32)[:, None]
    q_pos = blk * BLOCK + jnp.arange(BLOCK, dtype=jnp.int32)[None, :]
    k_pos = blk * BLOCK - BLOCK + jnp.arange(3 * BLOCK, dtype=jnp.int32)[None, :]
    valid = ((k_pos[:, None, :] >= 0) & (k_pos[:, None, :] < s)
             & (jnp.abs(k_pos[:, None, :] - q_pos[:, :, None]) <= WINDOW))
    scores = jnp.where(valid[None, :, None, None], scores, NEG_INF)
    sink_b = jnp.broadcast_to(sink.astype(jnp.float32).reshape(ATTN_KV_HEADS, ATTN_GROUP)[None, None, :, :, None, None],
                              scores.shape[:-1] + (1,))
    p = jax.nn.softmax(jnp.concatenate([scores, sink_b], axis=-1), axis=-1)[..., :-1]
    out = jnp.einsum('bnkgqs,bnskd->bnqkgd', p.astype(v.dtype), vb)
    return out.reshape(b, s, ATTN_Q_W)


def centred_depthwise_conv(t, w, bias):
    s = t.shape[1]
    tp = jnp.pad(t, ((0, 0), (CONV_LEFT, CONV_WIDTH - 1 - CONV_LEFT), (0, 0)))
    y = bias[None, None, :]
    for j in range(CONV_WIDTH):
        y = y + w[j][None, None, :] * tp[:, j:j + s]
    return y


def _lin_comb(c1, c2):
    a1, b1 = c1
    a2, b2 = c2
    return a1 * a2, a2 * b1 + b2


def bidir_rglru(xc, w_r, b_r, w_i, b_i, lam):
    b, s = xc.shape[0], xc.shape[1]
    xf = xc.astype(jnp.float32)
    xblk = xf.reshape(b, s, LRU_BLOCKS, LRU_BLOCK_W)
    r = jax.nn.sigmoid(jnp.einsum('bshc,dhce->dbshe', xblk, w_r.astype(jnp.float32)).reshape(2, b, s, LRU_WIDTH)
                       + b_r.astype(jnp.float32)[:, None, None, :])
    i = jax.nn.sigmoid(jnp.einsum('bshc,dhce->dbshe', xblk, w_i.astype(jnp.float32)).reshape(2, b, s, LRU_WIDTH)
                       + b_i.astype(jnp.float32)[:, None, None, :])
    log_a = -LRU_C * r * jax.nn.softplus(-lam.astype(jnp.float32))[:, None, None, :]
    a = jnp.exp(log_a)
    bx = jnp.sqrt(-jnp.expm1(2.0 * log_a)) * (i * xf[None])
    _, h_fwd = lax.associative_scan(_lin_comb, (a[0], bx[0]), axis=1)
    _, h_bwd = lax.associative_scan(_lin_comb, (a[1], bx[1]), axis=1, reverse=True)
    return (h_fwd + h_bwd).astype(xc.dtype)


def memory_cross_attention(q, k, v):
    scores = jnp.einsum('bshd,bmhd->bhsm', q, k).astype(jnp.float32) * (HEAD_DIM ** -0.5)
    p = jax.nn.softmax(scores, axis=-1)
    out = jnp.einsum('bhsm,bmhd->bshd', p.astype(v.dtype), v)
    return out.reshape(q.shape[0], q.shape[1], MEM_Q_W)


def peer_ffn(h, w_q, sub_keys, u_tab, v_tab):
    b, s, d = h.shape
    q = (h @ w_q).reshape(b, s, PEER_HEADS, 2, PEER_HALF)
    sc = jnp.einsum('bshpc,hpkc->bshpk', q, sub_keys).astype(jnp.float32)
    top_v, top_i = lax.top_k(sc, PEER_TOPK)
    cand_v = (top_v[..., 0, :, None] + top_v[..., 1, None, :]).reshape(b, s, PEER_HEADS, PEER_TOPK * PEER_TOPK)
    cand_i = (top_i[..., 0, :, None] * PEER_KEYS + top_i[..., 1, None, :]).reshape(b, s, PEER_HEADS, PEER_TOPK * PEER_TOPK)
    best_v, best_pos = lax.top_k(cand_v, PEER_TOPK)
    idx = jnp.take_along_axis(cand_i, best_pos, axis=-1)
    gate = jax.nn.softmax(best_v, axis=-1)
    n_tok = b * s
    n_chunk = n_tok // PEER_CHUNK
    hc = h.reshape(n_chunk, PEER_CHUNK, d)
    ic = idx.reshape(n_chunk, PEER_CHUNK, PEER_HEADS, PEER_TOPK)
    gc = gate.reshape(n_chunk, PEER_CHUNK, PEER_HEADS, PEER_TOPK)

    def expert_chunk(args):
        hx, ix, gx = args
        u = u_tab[ix]
        act = jax.nn.gelu(jnp.einsum('chkd,cd->chk', u, hx).astype(jnp.float32))
        vv = v_tab[ix]
        return jnp.einsum('chk,chkd->cd', (gx * act).astype(vv.dtype), vv)

    out = lax.map(expert_chunk, (hc, ic, gc))
    return out.reshape(b, s, d)


def setup_inputs(seed: int = 0) -> dict:
    key = jax.random.key(seed)
    ks = jax.random.split(key, 32)
    f32 = jnp.float32
    nrm = lambda k, shape, scale: jax.random.normal(k, shape, f32) * scale
    u = jax.random.uniform(ks[12], (DEPTH, 2, LRU_WIDTH), f32, 0.9, 0.999)
    a0 = u ** (1.0 / LRU_C)
    lam = jnp.log(a0) - jnp.log1p(-a0)
    return {
        "x": nrm(ks[0], (BATCH, SEQ, D_MODEL), 1.0),
        "mem": nrm(ks[1], (BATCH, MEM_LEN, D_MODEL), 1.0),
        "positions": jnp.broadcast_to(jnp.arange(SEQ, dtype=jnp.int32)[None, :], (BATCH, SEQ)),
        "g_mix": 1.0 + nrm(ks[2], (DEPTH, D_MODEL), 0.02),
        "g_mem": 1.0 + nrm(ks[3], (DEPTH, D_MODEL), 0.02),
        "w_in": nrm(ks[4], (DEPTH, D_MODEL, IN_W), D_MODEL ** -0.5),
        "gate_b": nrm(ks[5], (DEPTH, GATE_W), 0.02),
        "attn_sink": nrm(ks[6], (DEPTH, ATTN_HEADS), 0.5),
        "conv_w": nrm(ks[7], (DEPTH, CONV_WIDTH, LRU_WIDTH), CONV_WIDTH ** -0.5),
        "conv_b": nrm(ks[8], (DEPTH, LRU_WIDTH), 0.02),
        "lru_wr": nrm(ks[9], (DEPTH, 2, LRU_BLOCKS, LRU_BLOCK_W, LRU_BLOCK_W), LRU_BLOCK_W ** -0.5),
        "lru_br": nrm(ks[10], (DEPTH, 2, LRU_WIDTH), 0.02),
        "lru_wi": nrm(ks[11], (DEPTH, 2, LRU_BLOCKS, LRU_BLOCK_W, LRU_BLOCK_W), LRU_BLOCK_W ** -0.5),
        "lru_bi": nrm(ks[13], (DEPTH, 2, LRU_WIDTH), 0.02),
        "lru_lambda": lam,
        "w_mem_kv": nrm(ks[14], (DEPTH, D_MODEL, 2 * MEM_Q_W), D_MODEL ** -0.5),
        "p_attn": nrm(ks[15], (DEPTH, ATTN_Q_W, D_MODEL), ATTN_Q_W ** -0.5),
        "p_lru": nrm(ks[16], (DEPTH, LRU_WIDTH, D_MODEL), LRU_WIDTH ** -0.5),
        "p_mem": nrm(ks[17], (DEPTH, MEM_Q_W, D_MODEL), MEM_Q_W ** -0.5),
        "w_out": nrm(ks[18], (DEPTH, D_MODEL, D_MODEL), D_MODEL ** -0.5),
        "g_ffn": 1.0 + nrm(ks[19], (DEPTH, D_MODEL), 0.02),
        "w_peer_q": nrm(ks[20], (DEPTH, D_MODEL, PEER_HEADS * PEER_QDIM), D_MODEL ** -0.5),
        "peer_sub_keys": nrm(ks[21], (DEPTH, PEER_HEADS, 2, PEER_KEYS, PEER_HALF), PEER_HALF ** -0.5),
        "peer_u": nrm(ks[22], (DEPTH, PEER_EXPERTS, D_MODEL), D_MODEL ** -0.5),
        "peer_v": nrm(ks[23], (DEPTH, PEER_EXPERTS, D_MODEL), 0.25),
        "g_final": 1.0 + nrm(ks[24], (D_MODEL,), 0.02),
    }


def reference(x, mem, positions, g_mix, g_mem, w_in, gate_b, attn_sink, conv_w, conv_b,
              lru_wr, lru_br, lru_wi, lru_bi, lru_lambda, w_mem_kv, p_attn, p_lru, p_mem,
              w_out, g_ffn, w_peer_q, peer_sub_keys, peer_u, peer_v, g_final):
    b, s, d = x.shape
    offs = [int(o) for o in np.cumsum([ATTN_Q_W, ATTN_KV_W, ATTN_KV_W, LRU_WIDTH, LRU_WIDTH, MEM_Q_W])]
    for l in range(DEPTH):
        h = rmsnorm(x, g_mix[l])
        proj = h @ w_in[l]
        q_a, k_a, v_a, x_l, gate_l, q_m, gates = jnp.split(proj, offs, axis=-1)
        q_a = partial_rope(q_a.reshape(b, s, ATTN_HEADS, HEAD_DIM), positions)
        k_a = partial_rope(k_a.reshape(b, s, ATTN_KV_HEADS, HEAD_DIM), positions)
        v_a = v_a.reshape(b, s, ATTN_KV_HEADS, HEAD_DIM)
        attn_out = windowed_gqa(q_a, k_a, v_a, attn_sink[l])
        xc = centred_depthwise_conv(x_l, conv_w[l], conv_b[l])
        lru_out = bidir_rglru(xc, lru_wr[l], lru_br[l], lru_wi[l], lru_bi[l], lru_lambda[l]) * jax.nn.gelu(gate_l)
        kv_m = rmsnorm(mem, g_mem[l]) @ w_mem_kv[l]
        k_m = kv_m[..., :MEM_Q_W].reshape(b, MEM_LEN, MEM_HEADS, HEAD_DIM)
        v_m = kv_m[..., MEM_Q_W:].reshape(b, MEM_LEN, MEM_HEADS, HEAD_DIM)
        mem_out = memory_cross_attention(q_m.reshape(b, s, MEM_HEADS, HEAD_DIM), k_m, v_m)
        g = jax.nn.sigmoid(gates + gate_b[l]).reshape(b, s, N_BRANCH, d)
        merged = (g[:, :, 0] * (attn_out @ p_attn[l])
                  + g[:, :, 1] * (lru_out @ p_lru[l])
                  + g[:, :, 2] * (mem_out @ p_mem[l]))
        x = x + merged @ w_out[l]
        h2 = rmsnorm(x, g_ffn[l])
        x = x + peer_ffn(h2, w_peer_q[l], peer_sub_keys[l], peer_u[l], peer_v[l])
    return rmsnorm(x, g_final)
```

```python
import numpy as np
import concourse.bass as bass
import concourse.mybir as mybir
from concourse.bass_utils import run_bass_kernel_spmd

F32 = mybir.dt.float32
BF16 = mybir.dt.bfloat16
I32 = mybir.dt.int32
U32 = mybir.dt.uint32
AF = mybir.ActivationFunctionType
ALU = mybir.AluOpType
AX = mybir.AxisListType

N_CORES = 8
SEQ = 2048
DM = 1024
NSEQ_CORE = 2
OQ, OKK, OV, OXL, OGL, OQM, OG = 0, 768, 1024, 1280, 2048, 2816, 3328
EPS = 1e-6
TP = 256


class Buf:
    __slots__ = ("name", "w", "r", "excl")

    def __init__(self, name="", excl=False):
        self.name = name
        self.w = None
        self.r = []
        self.excl = excl


class Ins:
    __slots__ = ("eng", "seq", "fn", "signal", "cnt", "dma", "sem", "target", "waits")


class Sched:
    ENGS = ["pe", "act", "dve", "pool", "sp"]

    def __init__(self, nc, n_dma_sems=16, n_pool_sems=3):
        self.nc = nc
        self.prog = {e: [] for e in self.ENGS}
        self.known = {e: {} for e in self.ENGS}
        self.sem = {e: nc.alloc_semaphore("s_" + e) for e in self.ENGS}
        nq = {"sp": n_dma_sems, "pool": n_pool_sems}
        self.dsem = {q: [nc.alloc_semaphore(f"d_{q}{i}") for i in range(nq[q])] for q in ("sp", "pool")}
        self.dcount = {q: [0] * nq[q] for q in ("sp", "pool")}
        self.dlast = {q: [None] * nq[q] for q in ("sp", "pool")}
        self.drr = {"sp": 0, "pool": 0}

    def _need(self, eng, dep, waits):
        if dep is None:
            return
        if dep.dma:
            key = ("d", id(dep.sem))
            if self.known[eng].get(key, 0) >= dep.target:
                return
            self.known[eng][key] = dep.target
            waits.append(dep)
        else:
            if dep.eng == "pe" and eng == "pe":
                return
            key = dep.eng
            if self.known[eng].get(key, -1) >= dep.seq:
                return
            self.known[eng][key] = dep.seq
            dep.signal = True
            waits.append(dep)

    def _deps(self, eng, reads, writes):
        cands = []
        for b in reads:
            if b.w is not None:
                cands.append(b.w)
            if b.excl:
                cands.extend(r for r in b.r if r.eng != eng)
        for b in writes:
            if b.w is not None:
                cands.append(b.w)
            cands.extend(b.r)
        best = {}
        for d in cands:
            key = ("d", id(d.sem)) if d.dma else d.eng
            val = d.target if d.dma else d.seq
            if key not in best or val > best[key][0]:
                best[key] = (val, d)
        waits = []
        for key in best:
            self._need(eng, best[key][1], waits)
        return waits

    def _commit(self, ins, reads, writes):
        for b in reads:
            b.r.append(ins)
        for b in writes:
            b.w = ins
            b.r = []

    def _new(self, eng, fn):
        ins = Ins()
        ins.eng = eng
        ins.fn = fn
        ins.dma = False
        ins.signal = False
        ins.cnt = None
        ins.waits = []
        return ins

    def op(self, eng, fn, reads=(), writes=()):
        ins = self._new(eng, fn)
        ins.waits = self._deps(eng, reads, writes)
        ins.seq = len(self.prog[eng])
        self.prog[eng].append(ins)
        self._commit(ins, reads, writes)
        return ins

    def dma(self, q, fn, reads=(), writes=()):
        ins = self._new(q, fn)
        ins.dma = True
        ins.signal = True
        k = self.drr[q]
        self.drr[q] = (k + 1) % len(self.dsem[q])
        ins.sem = self.dsem[q][k]
        self.dcount[q][k] += 1
        ins.target = 16 * self.dcount[q][k]
        ins.waits = self._deps(q, reads, writes)
        prev = self.dlast[q][k]
        if prev is not None:
            self._need(q, prev, ins.waits)
        self.dlast[q][k] = ins
        ins.seq = len(self.prog[q])
        self.prog[q].append(ins)
        self._commit(ins, reads, writes)
        return ins

    def _lasts(self):
        lasts = []
        for e in self.ENGS:
            for i in reversed(self.prog[e]):
                if (not i.dma) and i.fn is not None:
                    lasts.append(i)
                    break
        for q in ("sp", "pool"):
            for i in self.dlast[q]:
                if i is not None:
                    lasts.append(i)
        return lasts

    def barrier(self, engines=None):
        lasts = self._lasts()
        for e in engines or self.ENGS:
            waits = []
            for d in lasts:
                if (not d.dma) and d.eng == e:
                    continue
                self._need(e, d, waits)
            if waits:
                ins = self._new(e, None)
                ins.waits = waits
                ins.seq = len(self.prog[e])
                self.prog[e].append(ins)

    def finish(self):
        self.barrier(engines=["sp"])

    def replay(self):
        nc = self.nc
        for e in self.ENGS:
            c = 0
            for i in self.prog[e]:
                if i.dma or i.fn is None:
                    continue
                if i.signal:
                    c += 1
                    i.cnt = c
        with nc.Block() as block:
            def run(engname, handle):
                for i in self.prog[engname]:
                    for d in i.waits:
                        if d.dma:
                            handle.wait_ge(d.sem, d.target)
                        else:
                            handle.wait_ge(self.sem[d.eng], d.cnt)
                    if i.fn is None:
                        continue
                    r = i.fn(handle)
                    if i.dma:
                        r.then_inc(i.sem, 16)
                    elif i.signal:
                        r.then_inc(self.sem[engname], 1)

            @block.tensor
            def _(t):
                run("pe", t)

            @block.scalar
            def _(s):
                run("act", s)

            @block.vector
            def _(v):
                run("dve", v)

            @block.gpsimd
            def _(g):
                run("pool", g)

            @block.sync
            def _(s):
                run("sp", s)


class Arena:
    ESZ = {F32: 4, BF16: 2, I32: 4, U32: 4}

    def __init__(self, ap, nwords):
        self.ap = ap
        self.n = nwords
        self.top = 0
        self.peak = 0

    def alloc(self, free_shape, dt, parts=128):
        n = int(np.prod(free_shape))
        nw = (n * self.ESZ[dt] + 3) // 4
        nw8 = (nw + 7) // 8 * 8
        o = self.top
        self.top += nw8
        self.peak = max(self.peak, self.top)
        assert self.top <= self.n, f"arena overflow {self.top*4} > {self.n*4}"
        v = self.ap[0:parts, o:o + nw]
        if dt != F32:
            v = v.bitcast(dt)
        if len(free_shape) == 2:
            v = v.rearrange("p (a b) -> p a b", a=free_shape[0])
        elif len(free_shape) == 3:
            v = v.rearrange("p (a b c) -> p a b c", a=free_shape[0], b=free_shape[1])
        return v

    def mark(self):
        return self.top

    def release(self, m):
        self.top = m


class Ring:
    def __init__(self, items):
        self.items = items
        self.i = 0

    def next(self):
        it = self.items[self.i]
        self.i = (self.i + 1) % len(self.items)
        return it


class TB:
    __slots__ = ("ap", "b")

    def __init__(self, ap, name="", excl=False):
        self.ap = ap
        self.b = Buf(name, excl)


def build_program(NSEQ=NSEQ_CORE, stop_after=None):
    nc = bass.Bass("TRN2", target_bir_lowering=False)
    NTOK = NSEQ * SEQ

    def din(name, shape, dt=F32):
        return nc.dram_tensor(name, list(shape), dt, kind="ExternalInput").ap()

    def dscr(name, shape, dt):
        return nc.dram_tensor(name, list(shape), dt).ap()

    x_d = din("x", [NSEQ, SEQ, DM])
    mem_d = din("mem", [NSEQ, 256, DM])
    pos_d = din("pos", [NSEQ, 128, SEQ], I32)
    win_h = din("w_in_h", [50, 128, 1024])
    wmkv_h = din("w_mkv_h", [8, 128, 1024])
    wpq_h = din("w_pq_h", [16, 128, 1024])
    pattn_h = din("p_attn_h", [8, 128, 768])
    plru_h = din("p_lru_h", [8, 128, 768])
    pmem_h = din("p_mem_h", [8, 128, 512])
    wout_h = din("w_out_h", [128, 8192])
    skT_h = din("skT_h", [128, 2048])
    lruw_h = din("lruw_h", [128, 3072])
    uT_h = din("uT_h", [128 * 128, 1024])
    v_h = din("v_h", [128 * 128, 1024])
    vec_h = din("vec_h", [128, 96])
    bc_h = din("bc_h", [128, 4, 1024])
    sink_h = din("sink_h", [128, 6])
    consts_h = din("consts_h", [128, 816])
    if stop_after in (None, "P1"):
        out_d = nc.dram_tensor("out", [NTOK, DM], F32, kind="ExternalOutput").ap()
        X1 = dscr("X1", [NTOK, DM], F32)
    else:
        X1 = nc.dram_tensor("out", [NTOK, DM], F32, kind="ExternalOutput").ap()
        out_d = None

    Win_s = dscr("Win_s", [50, 128, 1024], BF16)
    Wmkv_s = dscr("Wmkv_s", [8, 128, 1024], BF16)
    Wpq_s = dscr("Wpq_s", [16, 128, 1024], BF16)
    Pattn_s = dscr("Pattn_s", [8, 128, 768], BF16)
    Plru_s = dscr("Plru_s", [8, 128, 768], BF16)
    Pmem_s = dscr("Pmem_s", [8, 128, 512], BF16)
    UT_s = dscr("UT_s", [128 * 128, 1024], BF16)
    V_s = dscr("V_s", [128 * 128, 1024], BF16)

    S = Sched(nc)
    b_X1 = [Buf(f"X1_{i}") for i in range(NTOK // 128)]
    AW = 52992
    arena_t = nc.alloc_sbuf_tensor("arena", [128, AW], F32)
    ar = Arena(arena_t[:], AW)
    PS = [TB(nc.alloc_psum_tensor(f"ps{i}", [128, 512], F32)[:], f"ps{i}", True) for i in range(8)]
    PF = PS[0:6]
    PB = []
    for i in (6, 7):
        t_ = TB(PS[i].ap.bitcast(BF16), f"pb{i}")
        t_.b = PS[i].b
        PB.append(t_)
    pbr = Ring(PB)
    pfr = Ring(PF)

    def mm(out, lhsT, rhs, start, stop, r, w):
        S.op("pe", lambda e: e.matmul(out, lhsT=lhsT, rhs=rhs, start=start, stop=stop), r, w)

    def tr(out, in_, ident, r, w):
        S.op("pe", lambda e: e.transpose(out=out, in_=in_, identity=ident), r, w)

    def act(out, in_, func, r, w, bias=None, scale=1.0, accum=None):
        kw = {}
        if bias is not None:
            kw["bias"] = bias
        if accum is not None:
            kw["accum_out"] = accum
        S.op("act", lambda e: e.activation(out=out, in_=in_, func=func, scale=scale, **kw), r, w)

    def cp(eng, out, in_, r, w):
        if eng == "act":
            S.op("act", lambda e: e.copy(out=out, in_=in_), r, w)
        else:
            S.op(eng, lambda e: e.tensor_copy(out=out, in_=in_), r, w)

    def tt(eng, out, in0, in1, op, r, w):
        S.op(eng, lambda e: e.tensor_tensor(out=out, in0=in0, in1=in1, op=op), r, w)

    def ts(eng, out, in0, s1, s2, op0, op1, r, w):
        if s2 is None:
            S.op(eng, lambda e: e.tensor_scalar(out=out, in0=in0, scalar1=s1, scalar2=None, op0=op0), r, w)
        else:
            S.op(eng, lambda e: e.tensor_scalar(out=out, in0=in0, scalar1=s1, scalar2=s2, op0=op0, op1=op1), r, w)

    def stt(out, in0, scalar, in1, op0, op1, r, w):
        S.op("dve", lambda e: e.scalar_tensor_tensor(out=out, in0=in0, scalar=scalar, in1=in1, op0=op0, op1=op1), r, w)

    def red(out, in_, op, r, w):
        S.op("dve", lambda e: e.tensor_reduce(out=out, in_=in_, axis=AX.X, op=op), r, w)

    def recip(out, in_, r, w):
        S.op("dve", lambda e: e.reciprocal(out=out, in_=in_), r, w)

    def dma(q, out, in_, r, w, **kw):
        S.dma(q, lambda e: e.dma_start(out=out, in_=in_, **kw), r, w)

    def memset(eng, ap, val, w):
        S.op(eng, lambda e: e.memset(ap, val), (), w)

    b_Win = [Buf() for _ in range(50)]
    b_Wmkv, b_Wpq, b_Pattn, b_Plru, b_Pmem = Buf(), Buf(), Buf(), Buf(), Buf()
    b_UT = [Buf() for _ in range(32)]
    b_V = [Buf() for _ in range(32)]
    CK = dict(max_dma_last_dim=4096)

    consts = TB(ar.alloc((816,), F32), "consts")
    dma("sp", consts.ap, consts_h, [], [consts.b])
    identf = consts.ap[:, 0:128]
    mask = consts.ap[:, 160:544]
    iota128 = consts.ap[:, 544:672]
    iota16 = consts.ap[:, 672:688]
    identb = TB(ar.alloc((128,), BF16), "identb")
    dma("pool", identb.ap, consts_h[:, 0:128], [], [identb.b])
    permb = TB(ar.alloc((128,), BF16), "permb")
    dma("pool", permb.ap, consts_h[:, 688:816], [], [permb.b])
    maskb = TB(ar.alloc((384,), BF16), "maskb")
    dma("pool", maskb.ap, consts_h[:, 160:544], [], [maskb.b])
    vec = TB(ar.alloc((96,), F32), "vec")
    dma("sp", vec.ap, vec_h, [], [vec.b])
    sinkb = TB(ar.alloc((8,), F32), "sinkb")
    dma("sp", sinkb.ap[:, 0:6], sink_h, [], [sinkb.b])
    smalls = TB(ar.alloc((8,), F32), "smalls")
    memset("pool", smalls.ap[:, 0:1], EPS, [smalls.b])
    memset("pool", smalls.ap[:, 1:2], 1.0, [smalls.b])
    eps_t = smalls.ap[:, 0:1]
    one_t = smalls.ap[:, 1:2]
    lamv = TB(ar.alloc((16,), F32), "lamv")
    CW, CB, BR, BI, LAM, GB, INVF = 0, 24, 30, 42, 54, 66, 90

    def conv_win(g):
        dma("pool", Win_s[g * 5:(g + 1) * 5].rearrange("a p f -> (a p) f"),
            win_h[g * 5:(g + 1) * 5].rearrange("a p f -> (a p) f"), [], [b_Win[g * 5 + k] for k in range(5)], **CK)

    dma("pool", Wmkv_s.rearrange("a p f -> (a p) f"), wmkv_h.rearrange("a p f -> (a p) f"), [], [b_Wmkv], **CK)
    for g in (2, 3, 4, 0, 1):
        conv_win(g)

    def late_conv():
        for g in (5, 6, 7, 8, 9):
            conv_win(g)
        late_conv_rest()

    def late_conv_rest():
        dma("pool", Pattn_s.rearrange("a p f -> (a p) f"), pattn_h.rearrange("a p f -> (a p) f"), [], [b_Pattn], **CK)
        dma("pool", Plru_s.rearrange("a p f -> (a p) f"), plru_h.rearrange("a p f -> (a p) f"), [], [b_Plru], **CK)
        dma("pool", Pmem_s.rearrange("a p f -> (a p) f"), pmem_h.rearrange("a p f -> (a p) f"), [], [b_Pmem], **CK)
        for g in range(2):
            dma("pool", Wpq_s[g * 8:(g + 1) * 8].rearrange("a p f -> (a p) f"),
                wpq_h[g * 8:(g + 1) * 8].rearrange("a p f -> (a p) f"), [], [b_Wpq], **CK)

    def table_conv():
        for g in range(32):
            dma("pool", UT_s[g * 512:(g + 1) * 512, :], uT_h[g * 512:(g + 1) * 512, :], [], [b_UT[g]], **CK)
            yield
            dma("pool", V_s[g * 512:(g + 1) * 512, :], v_h[g * 512:(g + 1) * 512, :], [], [b_V[g]], **CK)
            yield

    tconv = table_conv()

    def conv_some(n):
        if stop_after not in (None, "P1"):
            return
        for _ in range(n):
            try:
                next(tconv)
            except StopIteration:
                return

    mtmp = ar.mark()
    t_e = TB(ar.alloc((16,), F32)); t_u = TB(ar.alloc((16,), F32)); t_l = TB(ar.alloc((16,), F32))
    lam_ap = vec.ap[:, LAM:LAM + 12]
    act(t_e.ap[:, 0:12], lam_ap, AF.Exp, [vec.b], [t_e.b], scale=-1.0)
    ts("dve", t_u.ap[:, 0:12], t_e.ap[:, 0:12], 1.0, None, ALU.add, None, [t_e.b], [t_u.b])
    act(t_l.ap[:, 0:12], t_u.ap[:, 0:12], AF.Ln, [t_u.b], [t_l.b])
    ts("dve", t_u.ap[:, 0:12], t_u.ap[:, 0:12], -1.0, 1e-30, ALU.add, ALU.max, [t_u.b], [t_u.b])
    recip(t_u.ap[:, 0:12], t_u.ap[:, 0:12], [t_u.b], [t_u.b])
    tt("dve", t_l.ap[:, 0:12], t_l.ap[:, 0:12], t_u.ap[:, 0:12], ALU.mult, [t_l.b, t_u.b], [t_l.b])
    tt("dve", t_l.ap[:, 0:12], t_l.ap[:, 0:12], t_e.ap[:, 0:12], ALU.mult, [t_l.b, t_e.b], [t_l.b])
    ts("dve", lamv.ap[:, 0:12], t_l.ap[:, 0:12], -8.0, None, ALU.mult, None, [t_l.b], [lamv.b])

    SCALE = float(128 ** -0.5)
    m_phase = ar.mark()

    def early_exit():
        S.finish()
        S.replay()
        return nc

    if stop_after == "P0":
        return early_exit()

    hT = ar.alloc((8, SEQ), BF16)
    b_hT = [Buf(f"hT{i}") for i in range(4)]
    kT = TB(ar.alloc((2, SEQ), BF16), "kT")
    vtok = TB(ar.alloc((16, 256), BF16), "vtok")
    lruT = ar.alloc((6, SEQ), BF16)
    b_lruT = [Buf(f"lruT{i}") for i in range(6)]
    Ct = TB(ar.alloc((SEQ,), F32), "C")
    St = TB(ar.alloc((SEQ,), F32), "S")
    kmT = TB(ar.alloc((4, 256), BF16), "kmT")
    vm = TB(ar.alloc((2, 512), BF16), "vm")
    wout = TB(ar.alloc((8, 1024), BF16), "wout")
    slabs = Ring([TB(ar.alloc((1024,), BF16), f"slab{i}") for i in range(8)])
    dma("pool", wout.ap.rearrange("p a b -> p (a b)"), wout_h, [], [wout.b], **CK)
    r_mark = ar.mark()

    def load_slab(src_ap, src_bufs, n=1024):
        sl = slabs.next()
        dma("sp", sl.ap[:, 0:n], src_ap, src_bufs, [sl.b])
        return sl

    def proj_fm(fc_src_ap, src_bufs, rhs_fn, rhs_bufs, ncols, nk=8):
        sl = load_slab(fc_src_ap, src_bufs, nk * 128)
        bank = pfr.next()
        for kc in range(nk):
            mm(bank.ap[:, 0:ncols], sl.ap[:, kc * 128:(kc + 1) * 128], rhs_fn(kc), kc == 0, kc == nk - 1,
               [sl.b] + rhs_bufs, [bank.b])
        return bank

    def rmsnorm_T(src_rows_ap, gb_ap, gb_buf, xt, xn, stat, dstT_ap, dst_bufs, cp_eng, src_bufs=()):
        dma("sp", xt.ap, src_rows_ap, list(src_bufs), [xt.b])
        act(xn.ap, xt.ap, AF.Square, [xt.b], [xn.b, stat.b], accum=stat.ap[:, 0:1])
        act(stat.ap[:, 1:2], stat.ap[:, 0:1], AF.Sqrt, [stat.b, smalls.b], [stat.b], bias=eps_t, scale=1.0 / DM)
        recip(stat.ap[:, 2:3], stat.ap[:, 1:2], [stat.b], [stat.b])
        stt(xn.ap, xt.ap, stat.ap[:, 2:3], gb_ap, ALU.mult, ALU.mult, [xt.b, stat.b, gb_buf], [xn.b])
        pb = pbr.next()
        for dc in range(8):
            tr(pb.ap[:, dc * 128:(dc + 1) * 128], xn.ap[:, dc * 128:(dc + 1) * 128], identb.ap, [xn.b, identb.b], [pb.b])
        cp(cp_eng, dstT_ap, pb.ap.rearrange("p (a b) -> p a b", a=8), [pb.b], dst_bufs)

    def range_reduce_sin(out_tb, ang_tb, tmpf, tmpi, n, parts=128):
        TWO_PI = float(2 * np.pi)
        a = ang_tb.ap[0:parts, 0:n]
        kf = tmpf.ap[0:parts, 0:n]
        ki = tmpi.ap[0:parts, 0:n]
        ts("dve", kf, a, 1.0 / TWO_PI, None, ALU.mult, None, [ang_tb.b], [tmpf.b])
        cp("dve", ki, kf, [tmpf.b], [tmpi.b])
        cp("dve", kf, ki, [tmpi.b], [tmpf.b])
        stt(a, kf, -TWO_PI, a, ALU.mult, ALU.add, [tmpf.b, ang_tb.b], [ang_tb.b])
        ts("dve", kf, a, float(np.pi), -TWO_PI, ALU.is_gt, ALU.mult, [ang_tb.b], [tmpf.b])
        tt("dve", a, a, kf, ALU.add, [ang_tb.b, tmpf.b], [ang_tb.b])
        ts("dve", kf, a, float(-np.pi), TWO_PI, ALU.is_lt, ALU.mult, [ang_tb.b], [tmpf.b])
        tt("dve", a, a, kf, ALU.add, [ang_tb.b, tmpf.b], [ang_tb.b])
        act(out_tb.ap[0:parts, 0:n], a, AF.Sin, [ang_tb.b], [out_tb.b])

    def rope_evac(bank, dst_ap, dst_bufs, tok0, n, tmpb, tmpa, tmpc):
        import os
        nst = int(os.environ.get("ROPE_STEPS", "9"))
        cp("act", tmpb.ap[:, 0:n], bank.ap[:, 0:n], [bank.b], [tmpb.b])
        if nst < 2: return
        pbk = pfr.next()
        mm(pbk.ap[:, 0:n], permb.ap, tmpb.ap[:, 0:n], True, True, [permb.b, tmpb.b], [pbk.b])
        if nst < 3: return
        tt("dve", tmpa.ap[:, 0:n], bank.ap[:, 0:n], Ct.ap[:, tok0:tok0 + n], ALU.mult, [bank.b, Ct.b], [tmpa.b])
        if nst < 4: return
        tt("dve", tmpc.ap[:, 0:n], pbk.ap[:, 0:n], St.ap[:, tok0:tok0 + n], ALU.mult, [pbk.b, St.b], [tmpc.b])
        if nst < 5: return
        tt("dve", dst_ap, tmpa.ap[:, 0:n], tmpc.ap[:, 0:n], ALU.add, [tmpa.b, tmpc.b], dst_bufs)

    for s in range(NSEQ):
        if s > 0:
            S.barrier()
        ar.release(r_mark)
        gmix = TB(ar.alloc((1024,), F32), "gmix")
        gmem = TB(ar.alloc((1024,), F32), "gmem")
        dma("sp", gmix.ap, bc_h[:, 0, :], [], [gmix.b])
        dma("sp", gmem.ap, bc_h[:, 1, :], [], [gmem.b])
        xts = [TB(ar.alloc((1024,), F32), f"xt{i}") for i in range(4)]
        xns = [TB(ar.alloc((1024,), BF16), f"xn{i}") for i in range(4)]
        stats = [TB(ar.alloc((8,), F32), f"st{i}") for i in range(4)]
        memT = TB(ar.alloc((8, 256), BF16), "memT")
        posi = TB(ar.alloc((SEQ,), I32), "posi")
        ang = TB(ar.alloc((SEQ,), F32), "ang")
        ang2 = TB(ar.alloc((SEQ,), F32), "ang2")
        tmpf = TB(ar.alloc((SEQ,), F32), "tmpf")
        tmpi = TB(ar.alloc((SEQ,), I32), "tmpi")
        for tb in range(16):
            k = tb % 4
            rmsnorm_T(x_d[s, tb * 128:(tb + 1) * 128, :], gmix.ap, gmix.b, xts[k], xns[k], stats[k],
                      hT[:, :, tb * 128:(tb + 1) * 128], [b_hT[tb // 4]], "act" if k else "dve")
            if stop_after == "M0a":
                return early_exit()
        if stop_after == "M0b":
            return early_exit()
        for mb in range(2):
            rmsnorm_T(mem_d[s, mb * 128:(mb + 1) * 128, :], gmem.ap, gmem.b, xts[mb], xns[mb], stats[mb],
                      memT.ap[:, :, mb * 128:(mb + 1) * 128], [memT.b], "act" if mb else "dve")
        dma("sp", posi.ap, pos_d[s], [], [posi.b])
        cp("dve", ang.ap, posi.ap, [posi.b], [ang.b])
        ts("dve", ang.ap, ang.ap, vec.ap[:, INVF:INVF + 1], None, ALU.mult, None, [ang.b, vec.b], [ang.b])
        ts("dve", ang2.ap, ang.ap, float(np.pi / 2), None, ALU.add, None, [ang.b], [ang2.b])
        range_reduce_sin(St, ang, tmpf, tmpi, SEQ)
        range_reduce_sin(Ct, ang2, tmpf, tmpi, SEQ)
        if stop_after == "M0c":
            return early_exit()
        for hd in range(4):
            bank = proj_fm(Wmkv_s[hd], [b_Wmkv], lambda kc: memT.ap[:, kc, :], [memT.b], 256)
            cp("act", kmT.ap[:, hd, :], bank.ap[:, 0:256], [bank.b], [kmT.b])
            if stop_after == "M0d":
                return early_exit()
        if stop_after == "M0e":
            return early_exit()
        for hd in range(4):
            sl = load_slab(Wmkv_s[4 + hd], [b_Wmkv])
            for mb in range(2):
                bank = pfr.next()
                for kc in range(8):
                    mm(bank.ap[:, 0:128], memT.ap[:, kc, mb * 128:(mb + 1) * 128], sl.ap[:, kc * 128:(kc + 1) * 128],
                       kc == 0, kc == 7, [memT.b, sl.b], [bank.b])
                cp("dve", vm.ap[:, mb, hd * 128:(hd + 1) * 128], bank.ap[:, 0:128], [bank.b], [vm.b])
        conv_some(4 if s else 0)
        if stop_after == "M0":
            return early_exit()

        S.barrier()
        ar.release(r_mark)
        lruw = TB(ar.alloc((24, 128), BF16), "lruw")
        dma("pool", lruw.ap.rearrange("p a b -> p (a b)"), lruw_h, [], [lruw.b], **CK)
        if s == 0:
            late_conv()
        T = [TB(ar.alloc((SEQ + 8,), F32), f"T{i}") for i in range(6)]
        xcb = TB(ar.alloc((SEQ,), BF16), "xcb")
        for cb in range(6):
            xl, xc, t2, t3, t4, t5 = T
            memset("pool", xl.ap[:, 0:2], 0.0, [xl.b])
            memset("pool", xl.ap[:, SEQ + 2:SEQ + 3], 0.0, [xl.b])
            sl = load_slab(Win_s[OXL // 128 + cb], [b_Win[OXL // 128 + cb]])
            for tc in range(4):
                bank = pfr.next()
                for kc in range(8):
                    mm(bank.ap, sl.ap[:, kc * 128:(kc + 1) * 128], hT[:, kc, tc * 512:(tc + 1) * 512], kc == 0, kc == 7,
                       [sl.b, b_hT[tc]], [bank.b])
                cp("act" if tc % 2 else "dve", xl.ap[:, 2 + tc * 512:2 + (tc + 1) * 512], bank.ap, [bank.b], [xl.b])
            cw = lambda j: vec.ap[:, CW + cb * 4 + j:CW + cb * 4 + j + 1]
            ts("dve", xc.ap[:, 0:SEQ], xl.ap[:, 0:SEQ], cw(0), vec.ap[:, CB + cb:CB + cb + 1], ALU.mult, ALU.add,
               [xl.b, vec.b], [xc.b])
            for j in range(1, 4):
                stt(xc.ap[:, 0:SEQ], xl.ap[:, j:j + SEQ], cw(j), xc.ap[:, 0:SEQ], ALU.mult, ALU.add, [xl.b, vec.b, xc.b], [xc.b])
            cp("act", xcb.ap, xc.ap[:, 0:SEQ], [xc.b], [xcb.b])
            for d in range(2):
                col = d * 6 + cb
                for (which, dst, bo) in ((0, t2, BR), (1, t3, BI)):
                    for tc in range(4):
                        bank = pfr.next()
                        mm(bank.ap, lruw.ap[:, which * 12 + col, :], xcb.ap[:, tc * 512:(tc + 1) * 512], True, True,
                           [lruw.b, xcb.b], [bank.b])
                        act(dst.ap[:, tc * 512:(tc + 1) * 512], bank.ap, AF.Sigmoid, [bank.b, vec.b], [dst.b],
                            bias=vec.ap[:, bo + col:bo + col + 1])
                a_ap = t2.ap[:, 0:SEQ]
                act(a_ap, a_ap, AF.Exp, [t2.b, lamv.b], [t2.b], scale=lamv.ap[:, col:col + 1])
                stt(t4.ap[:, 0:SEQ], a_ap, -1.0, a_ap, ALU.mult, ALU.mult, [t2.b], [t4.b])
                act(t4.ap[:, 0:SEQ], t4.ap[:, 0:SEQ], AF.Sqrt, [t4.b, smalls.b], [t4.b], bias=one_t)
                tt("dve", t3.ap[:, 0:SEQ], t3.ap[:, 0:SEQ], t4.ap[:, 0:SEQ], ALU.mult, [t3.b, t4.b], [t3.b])
                tt("dve", t3.ap[:, 0:SEQ], t3.ap[:, 0:SEQ], xc.ap[:, 0:SEQ], ALU.mult, [t3.b, xc.b], [t3.b])
                if d == 0:
                    S.op("dve", lambda e, o=t5.ap[:, 0:SEQ], a0=a_ap, b0=t3.ap[:, 0:SEQ]: e.tensor_tensor_scan(
                        out=o, data0=a0, data1=b0, initial=0.0, op0=ALU.mult, op1=ALU.add), [t2.b, t3.b], [t5.b])
                else:
                    S.op("dve", lambda e, o=t4.ap[:, SEQ - 1::-1], a0=t2.ap[:, SEQ - 1::-1], b0=t3.ap[:, SEQ - 1::-1]:
                         e.tensor_tensor_scan(out=o, data0=a0, data1=b0, initial=0.0, op0=ALU.mult, op1=ALU.add),
                         [t2.b, t3.b], [t4.b])
            tt("dve", t5.ap[:, 0:SEQ], t5.ap[:, 0:SEQ], t4.ap[:, 0:SEQ], ALU.add, [t5.b, t4.b], [t5.b])
            sl = load_slab(Win_s[OGL // 128 + cb], [b_Win[OGL // 128 + cb]])
            for tc in range(4):
                bank = pfr.next()
                for kc in range(8):
                    mm(bank.ap, sl.ap[:, kc * 128:(kc + 1) * 128], hT[:, kc, tc * 512:(tc + 1) * 512], kc == 0, kc == 7,
                       [sl.b, b_hT[tc]], [bank.b])
                act(xl.ap[:, tc * 512:(tc + 1) * 512], bank.ap, AF.Gelu_apprx_tanh, [bank.b], [xl.b])
            tt("dve", lruT[:, cb, :], xl.ap[:, 0:SEQ], t5.ap[:, 0:SEQ], ALU.mult, [xl.b, t5.b], [b_lruT[cb]])
            conv_some(2)

        if stop_after == "M1":
            return early_exit()
        S.barrier()
        ar.release(r_mark)
        qT = TB(ar.alloc((6, 512), BF16), "qT")
        qmT = TB(ar.alloc((4, 512), BF16), "qmT")
        aoT = TB(ar.alloc((6, 512), BF16), "aoT")
        moT = TB(ar.alloc((4, 512), BF16), "moT")
        mgT = TB(ar.alloc((8, 512), BF16), "mgT")
        gts = Ring([TB(ar.alloc((512,), F32), f"g{i}") for i in range(2)])
        accm = TB(ar.alloc((512,), F32), "accm")
        tmpm = Ring([TB(ar.alloc((512,), F32), f"tmpm{i}") for i in range(1)])
        xres = Ring([TB(ar.alloc((1024,), F32), f"xres{i}") for i in range(4)])
        rtb = Ring([TB(ar.alloc((512,), BF16), f"rtb{i}") for i in range(2)])
        rta = Ring([TB(ar.alloc((512,), F32), f"rta{i}") for i in range(1)])
        rtc = Ring([TB(ar.alloc((512,), F32), f"rtc{i}") for i in range(1)])
        NH = 6
        s_sb = [TB(ar.alloc((384,), F32), f"s{i}") for i in range(NH)]
        Pn = [TB(ar.alloc((384,), BF16), f"Pn{i}") for i in range(NH)]
        PT = [TB(ar.alloc((384,), BF16), f"PT{i}") for i in range(NH)]
        sm = [TB(ar.alloc((8,), F32), f"sm{i}") for i in range(NH)]

        for kvh in range(2):
            for tc in range(4):
                bank = proj_fm(Win_s[OKK // 128 + kvh], [b_Win[OKK // 128 + kvh]],
                               lambda kc: hT[:, kc, tc * 512:(tc + 1) * 512], [b_hT[tc]], 512)
                if stop_after == "M2p":
                    return early_exit()
                rope_evac(bank, kT.ap[:, kvh, tc * 512:(tc + 1) * 512], [kT.b], tc * 512, 512, rtb.next(), rta.next(), rtc.next())
                if stop_after == "M2a":
                    return early_exit()
        if stop_after == "M2b":
            return early_exit()
        for kvh in range(2):
            sl = load_slab(Win_s[OV // 128 + kvh], [b_Win[OV // 128 + kvh]])
            for tb in range(16):
                bank = pfr.next()
                for kc in range(8):
                    mm(bank.ap[:, 0:128], hT[:, kc, tb * 128:(tb + 1) * 128], sl.ap[:, kc * 128:(kc + 1) * 128],
                       kc == 0, kc == 7, [b_hT[tb // 4], sl.b], [bank.b])
                cp("act" if tb % 2 else "dve", vtok.ap[:, tb, kvh * 128:(kvh + 1) * 128], bank.ap[:, 0:128], [bank.b], [vtok.b])
        conv_some(4)
        if stop_after == "M2":
            return early_exit()

        def softmax_block(nh, score_fn, NK, mask_ap, sink_col, v_fn, nkb, out_fn):
            banks = []
            for i in range(nh):
                bank = pfr.next()
                score_fn(i, bank, mask_ap is None)
                if mask_ap is not None:
                    mm(bank.ap[:, 0:NK], identb.ap, mask_ap, False, True, [identb.b, maskb.b], [bank.b])
                banks.append(bank)
            for i in range(nh):
                red(sm[i].ap[:, 0:1], banks[i].ap[:, 0:NK], ALU.max, [banks[i].b], [sm[i].b])
                if sink_col is not None:
                    ts("dve", sm[i].ap[:, 0:1], sm[i].ap[:, 0:1], SCALE, sinkb.ap[:, sink_col(i):sink_col(i) + 1], ALU.mult, ALU.max,
                       [sm[i].b, sinkb.b], [sm[i].b])
                    ts("dve", sm[i].ap[:, 1:2], sm[i].ap[:, 0:1], -1.0, None, ALU.mult, None, [sm[i].b], [sm[i].b])
                else:
                    ts("dve", sm[i].ap[:, 1:2], sm[i].ap[:, 0:1], -SCALE, None, ALU.mult, None, [sm[i].b], [sm[i].b])
            for i in range(nh):
                sa = s_sb[i].ap[:, 0:NK]
                act(sa, banks[i].ap[:, 0:NK], AF.Exp, [banks[i].b, sm[i].b], [s_sb[i].b, sm[i].b], bias=sm[i].ap[:, 1:2], scale=SCALE,
                    accum=sm[i].ap[:, 2:3])
                if sink_col is not None:
                    act(sm[i].ap[:, 3:4], sm[i].ap[:, 1:2], AF.Exp, [sm[i].b, sinkb.b], [sm[i].b],
                        bias=sinkb.ap[:, sink_col(i):sink_col(i) + 1])
            for i in range(nh):
                sa = s_sb[i].ap[:, 0:NK]
                if sink_col is not None:
                    tt("dve", sm[i].ap[:, 4:5], sm[i].ap[:, 2:3], sm[i].ap[:, 3:4], ALU.add, [sm[i].b], [sm[i].b])
                    recip(sm[i].ap[:, 5:6], sm[i].ap[:, 4:5], [sm[i].b], [sm[i].b])
                else:
                    recip(sm[i].ap[:, 5:6], sm[i].ap[:, 2:3], [sm[i].b], [sm[i].b])
                ts("dve", Pn[i].ap[:, 0:NK], sa, sm[i].ap[:, 5:6], None, ALU.mult, None, [s_sb[i].b, sm[i].b], [Pn[i].b])
            pbs = []
            for i in range(nh):
                pb = pbr.next()
                for j in range(nkb):
                    tr(pb.ap[:, j * 128:(j + 1) * 128], Pn[i].ap[:, j * 128:(j + 1) * 128], identb.ap, [Pn[i].b, identb.b], [pb.b])
                cp("act", PT[i].ap[:, 0:NK], pb.ap[:, 0:NK], [pb.b], [PT[i].b])
            for i in range(nh):
                ob = pfr.next()
                for j in range(nkb):
                    vap, vb = v_fn(i, j)
                    mm(ob.ap[:, 0:128], vap, PT[i].ap[:, j * 128:(j + 1) * 128], j == 0, j == nkb - 1, vb + [PT[i].b], [ob.b])
                dst, db = out_fn(i)
                cp("act", dst, ob.ap[:, 0:128], [ob.b], db)

        for tc in range(4):
            t0 = tc * 512
            hb = [b_hT[tc]]
            rhs_h = lambda kc: hT[:, kc, t0:t0 + 512]
            for h in range(6):
                bank = proj_fm(Win_s[OQ // 128 + h], [b_Win[OQ // 128 + h]], rhs_h, hb, 512)
                rope_evac(bank, qT.ap[:, h, :], [qT.b], t0, 512, rtb.next(), rta.next(), rtc.next())
            for hd in range(4):
                bank = proj_fm(Win_s[OQM // 128 + hd], [b_Win[OQM // 128 + hd]], rhs_h, hb, 512)
                cp("act", qmT.ap[:, hd, :], bank.ap, [bank.b], [qmT.b])
            for qb in range(4):
                n = tc * 4 + qb
                kb0, kb1 = max(0, n - 1), min(15, n + 1)
                nkb = kb1 - kb0 + 1
                NK = nkb * 128
                moff = 0 if n > 0 else 128
                qs = slice(qb * 128, (qb + 1) * 128)

                def score_a(i, bank, stop, qs=qs, kb0=kb0, NK=NK):
                    mm(bank.ap[:, 0:NK], qT.ap[:, i, qs], kT.ap[:, i // 3, kb0 * 128:kb0 * 128 + NK], True, stop,
                       [qT.b, kT.b], [bank.b])

                softmax_block(6, score_a, NK, maskb.ap[:, moff:moff + NK], lambda i: i,
                              lambda i, j, kb0=kb0: (vtok.ap[:, kb0 + j, (i // 3) * 128:(i // 3 + 1) * 128], [vtok.b]),
                              nkb, lambda i, qs=qs: (aoT.ap[:, i, qs], [aoT.b]))

                def score_m(i, bank, stop, qs=qs):
                    mm(bank.ap[:, 0:256], qmT.ap[:, i, qs], kmT.ap[:, i, :], True, stop, [qmT.b, kmT.b], [bank.b])

                softmax_block(4, score_m, 256, None, None,
                              lambda i, j: (vm.ap[:, j, i * 128:(i + 1) * 128], [vm.b]),
                              2, lambda i, qs=qs: (moT.ap[:, i, qs], [moT.b]))
            xrs = []
            for tb in range(4):
                xr = xres.next()
                dma("sp", xr.ap, x_d[s, t0 + tb * 128:t0 + (tb + 1) * 128, :], [], [xr.b])
                xrs.append(xr)
            for dmc in range(8):
                for b in range(3):
                    fc = OG // 128 + b * 8 + dmc
                    gbank = proj_fm(Win_s[fc], [b_Win[fc]], rhs_h, hb, 512)
                    gt = gts.next()
                    act(gt.ap, gbank.ap, AF.Sigmoid, [gbank.b, vec.b], [gt.b], bias=vec.ap[:, GB + b * 8 + dmc:GB + b * 8 + dmc + 1])
                    if b == 0:
                        pbank = proj_fm(Pattn_s[dmc], [b_Pattn], lambda kc: aoT.ap[:, kc, :], [aoT.b], 512, nk=6)
                    elif b == 1:
                        pbank = proj_fm(Plru_s[dmc], [b_Plru], lambda kc: lruT[:, kc, t0:t0 + 512], [b_lruT[kc2] for kc2 in range(6)], 512, nk=6)
                    else:
                        pbank = proj_fm(Pmem_s[dmc], [b_Pmem], lambda kc: moT.ap[:, kc, :], [moT.b], 512, nk=4)
                    if b == 0:
                        tt("dve", accm.ap, gt.ap, pbank.ap, ALU.mult, [gt.b, pbank.b], [accm.b])
                    elif b == 1:
                        tm = tmpm.next()
                        tt("dve", tm.ap, gt.ap, pbank.ap, ALU.mult, [gt.b, pbank.b], [tm.b])
                        tt("pool", accm.ap, accm.ap, tm.ap, ALU.add, [accm.b, tm.b], [accm.b])
                    else:
                        tm = tmpm.next()
                        tt("dve", tm.ap, gt.ap, pbank.ap, ALU.mult, [gt.b, pbank.b], [tm.b])
                        tt("pool", mgT.ap[:, dmc, :], accm.ap, tm.ap, ALU.add, [accm.b, tm.b], [mgT.b])
            for tb in range(4):
                row0 = s * SEQ + t0 + tb * 128
                xr = xrs[tb]
                for half in range(2):
                    bank = pfr.next()
                    for dmc in range(8):
                        mm(bank.ap, mgT.ap[:, dmc, tb * 128:(tb + 1) * 128], wout.ap[:, dmc, half * 512:(half + 1) * 512],
                           dmc == 0, dmc == 7, [mgT.b, wout.b], [bank.b])
                    tt("dve", xr.ap[:, half * 512:(half + 1) * 512], xr.ap[:, half * 512:(half + 1) * 512], bank.ap, ALU.add,
                       [xr.b, bank.b], [xr.b])
                dma("sp", X1[row0:row0 + 128, :], xr.ap, [xr.b], [b_X1[row0 // 128]])
            conv_some(6)

    if stop_after == "M":
        S.finish()
        S.replay()
        print("arena peak bytes", ar.peak * 4)
        return nc

    conv_some(1000)
    S.barrier()
    ar.release(m_phase)
    skT = TB(ar.alloc((16, 128), BF16), "skT")
    dma("pool", skT.ap.rearrange("p a b -> p (a b)"), skT_h, [], [skT.b], **CK)
    gffn = TB(ar.alloc((1024,), F32), "gffn")
    gfin = TB(ar.alloc((1024,), F32), "gfin")
    dma("sp", gffn.ap, bc_h[:, 2, :], [], [gffn.b])
    dma("sp", gfin.ap, bc_h[:, 3, :], [], [gfin.b])
    GT = TB(ar.alloc((128, TP), BF16), "GT")
    h2Ts = [TB(ar.alloc((8, TP), BF16), f"h2T{i}") for i in range(2)]
    x1ts = [[TB(ar.alloc((1024,), F32), f"x1t{k}{i}") for i in range(2)] for k in range(2)]
    trTs = [TB(ar.alloc((3, TP), BF16), f"trT{i}") for i in range(2)]
    iotab = TB(ar.alloc((128,), BF16), "iotab")
    dma("pool", iotab.ap, consts_h[:, 544:672], [], [iotab.b])
    xnr = Ring([TB(ar.alloc((1024,), BF16), f"xnp{i}") for i in range(2)])
    pst = Ring([TB(ar.alloc((8,), F32), f"pst{i}") for i in range(4)])
    qpT = TB(ar.alloc((16, TP), BF16), "qpT")
    sc = TB(ar.alloc((16, 128), F32), "sc")
    wk = ar.alloc((16, 128), F32)
    b_wk = [Buf(f"wk{i}") for i in range(16)]
    cand_ap = sc.ap.rearrange("p (h two) k -> p h (two k)", two=2)
    wk2_ap = wk.rearrange("p (h two) k -> p h (two k)", two=2)
    tv = ar.alloc((16, 16), F32)
    b_tv = [Buf(f"tv{i}") for i in range(16)]
    ti = ar.alloc((16, 16), U32)
    b_ti = [Buf(f"ti{i}") for i in range(16)]
    tif = TB(ar.alloc((16, 16), F32), "tif")
    bv = ar.alloc((8, 16), F32)
    b_bv = [Buf(f"bv{i}") for i in range(8)]
    bp = ar.alloc((8, 16), U32)
    b_bp = [Buf(f"bp{i}") for i in range(8)]
    bpa = TB(ar.alloc((8, 16), U32), "bpa")
    bpb = TB(ar.alloc((8, 16), U32), "bpb")
    abf = TB(ar.alloc((2, 8, 16), F32), "abf")
    ge = TB(ar.alloc((8, 16), F32), "ge")
    gs = TB(ar.alloc((16,), F32), "gs")
    oh = TB(ar.alloc((8, 16, 16), F32), "oh")
    idx3 = Ring([TB(ar.alloc((3, 128), F32), f"idx3{i}") for i in range(2)])
    TS = 8
    Jr = Ring([TB(ar.alloc((128 * TS,), BF16), f"J{i}") for i in range(3)])
    W0r = Ring([TB(ar.alloc((64 * TS,), BF16), f"W0{i}") for i in range(2)])
    Wr = Ring([TB(ar.alloc((64 * TS,), BF16), f"W{i}") for i in range(3)])
    iota_jt = TB(ar.alloc((128 * TS,), BF16), "iota_jt")
    ucr = Ring([TB(ar.alloc((1024,), BF16), f"uc{i}") for i in range(4)])
    vcr = Ring([TB(ar.alloc((1024,), BF16), f"vc{i}") for i in range(6)])
    gar = Ring([TB(ar.alloc((TP,), BF16), f"ga{i}") for i in range(4)])
    GAr = Ring([TB(ar.alloc((TP,), BF16), f"GA{i}") for i in range(4)])
    pslabs = Ring([TB(ar.alloc((1024,), BF16), f"pslab{i}") for i in range(4)])
    print("PEER arena top bytes", ar.top * 4)
    acc = PS[0:4]
    cbanks = Ring([PS[4], PS[5]])
    abanks = Ring([PS[6], PS[7]])
    cp("dve", iota_jt.ap.rearrange("p (j t) -> p j t", t=TS), iotab.ap.unsqueeze(2).to_broadcast([128, 128, TS]), [iotab.b], [iota_jt.b])
    io_jt = iota_jt.ap.rearrange("p (j t) -> p j t", t=TS)
    io16 = iota16.unsqueeze(1).unsqueeze(1).to_broadcast([128, 8, 16, 16])
    tv4 = tv.rearrange("p (h two) k -> p h two k", two=2)
    tif4 = tif.ap.rearrange("p (h two) k -> p h two k", two=2)
    NP = NTOK // TP
    if stop_after == "P1":
        NP = 1

    def dvop(fn, r, w):
        S.op("dve", fn, r, w)

    def stage_A(p):
        r0 = p * TP
        x1t, h2T, trT = x1ts[p % 2], h2Ts[p % 2], trTs[p % 2]
        xn_, st_ = [xnr.next(), xnr.next()], [pst.next(), pst.next()]
        sls = {}
        for tb in range(2):
            rows = slice(r0 + tb * 128, r0 + (tb + 1) * 128)
            dma("sp", x1t[tb].ap, X1[rows, :], [b_X1[(r0 + tb * 128) // 128]], [x1t[tb].b])
        for hp in range(2):
            sls[hp] = pslabs.next()
            dma("sp", sls[hp].ap, Wpq_s[hp], [b_Wpq], [sls[hp].b])
        yield
        for tb in range(2):
            act(xn_[tb].ap, x1t[tb].ap, AF.Square, [x1t[tb].b], [xn_[tb].b, st_[tb].b], accum=st_[tb].ap[:, 0:1])
            act(st_[tb].ap[:, 1:2], st_[tb].ap[:, 0:1], AF.Sqrt, [st_[tb].b, smalls.b], [st_[tb].b], bias=eps_t, scale=1.0 / DM)
        yield
        for tb in range(2):
            recip(st_[tb].ap[:, 2:3], st_[tb].ap[:, 1:2], [st_[tb].b], [st_[tb].b])
            stt(xn_[tb].ap, x1t[tb].ap, st_[tb].ap[:, 2:3], gffn.ap, ALU.mult, ALU.mult, [x1t[tb].b, st_[tb].b, gffn.b], [xn_[tb].b])
        yield
        pbs_ = []
        for tb in range(2):
            pb = pbr.next()
            for dc in range(8):
                tr(pb.ap[:, dc * 128:(dc + 1) * 128], xn_[tb].ap[:, dc * 128:(dc + 1) * 128], identb.ap, [xn_[tb].b, identb.b], [pb.b])
            pbs_.append(pb)
        yield
        for tb in range(2):
            cp("act", h2T.ap[:, :, tb * 128:(tb + 1) * 128], pbs_[tb].ap.rearrange("p (a b) -> p a b", a=8), [pbs_[tb].b], [h2T.b])
        yield
        prev_bank = None
        for hp in range(16 + 1):
            if hp + 2 < 16:
                sls[hp + 2] = pslabs.next()
                dma("sp", sls[hp + 2].ap, Wpq_s[hp + 2], [b_Wpq], [sls[hp + 2].b])
            if prev_bank is not None:
                cp("act", qpT.ap[:, hp - 1, :], prev_bank.ap[:, 0:TP], [prev_bank.b], [qpT.b])
                prev_bank = None
            if hp < 16:
                sl = sls.pop(hp)
                bank = abanks.next()
                for kc in range(8):
                    mm(bank.ap[:, 0:TP], sl.ap[:, kc * 128:(kc + 1) * 128], h2T.ap[:, kc, :], kc == 0, kc == 7, [sl.b, h2T.b], [bank.b])
                prev_bank = bank
            yield
        for tb in range(2):
            id3 = idx3.next()
            for g4 in range(4):
                bank = abanks.next()
                for q in range(4):
                    hp = g4 * 4 + q
                    mm(bank.ap[:, q * 128:(q + 1) * 128], qpT.ap[:, hp, tb * 128:(tb + 1) * 128], skT.ap[:, hp, :], True, True,
                       [qpT.b, skT.b], [bank.b])
                cp("act" if g4 % 2 else "dve", sc.ap[:, g4 * 4:(g4 + 1) * 4, :], bank.ap.rearrange("p (a b) -> p a b", a=4), [bank.b], [sc.b])
            yield
            for hp in range(16):
                dvop(lambda e, hp=hp: e.max(out=tv[:, hp, 0:8], in_=sc.ap[:, hp, :]), [sc.b], [b_tv[hp]])
            yield
            for hp in range(16):
                dvop(lambda e, hp=hp: e.max_index(out=ti[:, hp, 0:8], in_max=tv[:, hp, 0:8], in_values=sc.ap[:, hp, :]), [sc.b, b_tv[hp]], [b_ti[hp]])
            yield
            for hp in range(16):
                dvop(lambda e, hp=hp: e.match_replace(out=wk[:, hp, :], in_to_replace=tv[:, hp, 0:8], in_values=sc.ap[:, hp, :], imm_value=-1e30),
                     [sc.b, b_tv[hp]], [b_wk[hp]])
            yield
            for hp in range(16):
                dvop(lambda e, hp=hp: e.max(out=tv[:, hp, 8:16], in_=wk[:, hp, :]), [b_wk[hp]], [b_tv[hp]])
            yield
            for hp in range(16):
                dvop(lambda e, hp=hp: e.max_index(out=ti[:, hp, 8:16], in_max=tv[:, hp, 8:16], in_values=wk[:, hp, :]), [b_wk[hp], b_tv[hp]], [b_ti[hp]])
            yield
            cp("dve", tif.ap, ti, b_ti, [tif.b])
            tt("dve", cand_ap.rearrange("p h (a b) -> p h a b", a=16), tv4[:, :, 0, :].unsqueeze(3).to_broadcast([128, 8, 16, 16]),
               tv4[:, :, 1, :].unsqueeze(2).to_broadcast([128, 8, 16, 16]), ALU.add, b_tv + [sc.b], [sc.b])
            yield
            for h in range(8):
                dvop(lambda e, h=h: e.max(out=bv[:, h, 0:8], in_=cand_ap[:, h, :]), [sc.b], [b_bv[h]])
            for h in range(8):
                dvop(lambda e, h=h: e.max_index(out=bp[:, h, 0:8], in_max=bv[:, h, 0:8], in_values=cand_ap[:, h, :]), [sc.b, b_bv[h]], [b_bp[h]])
            yield
            for h in range(8):
                dvop(lambda e, h=h: e.match_replace(out=wk2_ap[:, h, :], in_to_replace=bv[:, h, 0:8], in_values=cand_ap[:, h, :], imm_value=-1e30),
                     [sc.b, b_bv[h]] + b_wk, [b_wk[2 * h], b_wk[2 * h + 1]])
            yield
            for h in range(8):
                dvop(lambda e, h=h: e.max(out=bv[:, h, 8:16], in_=wk2_ap[:, h, :]), [b_wk[2 * h], b_wk[2 * h + 1]], [b_bv[h]])
            for h in range(8):
                dvop(lambda e, h=h: e.max_index(out=bp[:, h, 8:16], in_max=bv[:, h, 8:16], in_values=wk2_ap[:, h, :]),
                     [b_wk[2 * h], b_wk[2 * h + 1], b_bv[h]], [b_bp[h]])
            yield
            tt("dve", ge.ap, bv, bv[:, :, 0:1].to_broadcast([128, 8, 16]), ALU.subtract, b_bv, [ge.b])
            act(ge.ap, ge.ap, AF.Exp, [ge.b], [ge.b])
            red(gs.ap[:, 0:8], ge.ap, ALU.add, [ge.b], [gs.b])
            recip(gs.ap[:, 8:16], gs.ap[:, 0:8], [gs.b], [gs.b])
            tt("dve", id3.ap[:, 2, :].rearrange("p (h k) -> p h k", h=8), ge.ap, gs.ap[:, 8:16].unsqueeze(2).to_broadcast([128, 8, 16]), ALU.mult,
               [ge.b, gs.b], [id3.b])
            yield
            dvop(lambda e: e.tensor_single_scalar(out=bpa.ap, in_=bp, scalar=4, op=ALU.logical_shift_right), b_bp, [bpa.b])
            dvop(lambda e: e.tensor_single_scalar(out=bpb.ap, in_=bp, scalar=15, op=ALU.bitwise_and), b_bp, [bpb.b])
            cp("dve", abf.ap[:, 0, :, :], bpa.ap, [bpa.b], [abf.b])
            cp("dve", abf.ap[:, 1, :, :], bpb.ap, [bpb.b], [abf.b])
            yield
            for half in range(2):
                tt("dve", oh.ap, abf.ap[:, half, :, :].unsqueeze(3).to_broadcast([128, 8, 16, 16]), io16, ALU.is_equal, [abf.b, consts.b], [oh.b])
                tt("dve", oh.ap, oh.ap, tif4[:, :, half, :].unsqueeze(2).to_broadcast([128, 8, 16, 16]), ALU.mult, [oh.b, tif.b], [oh.b])
                red(id3.ap[:, half, :].rearrange("p (h k) -> p h k", h=8), oh.ap, ALU.add, [oh.b], [id3.b])
                yield
            yield
            for q in range(3):
                bank = abanks.next()
                tr(bank.ap[:, 0:128], id3.ap[:, q, :], identf, [id3.b, consts.b], [bank.b])
                cp("act" if q % 2 else "dve", trT.ap[:, q, tb * 128:(tb + 1) * 128], bank.ap[:, 0:128], [bank.b], [trT.b])
            yield

    b_GT = [Buf("GT_lo"), Buf("GT_hi")]

    def stage_B(p, h):
        trT = trTs[p % 2]
        i0 = h * 64
        NSB = TP // TS
        ctx = {}
        for k in range(NSB + 2):
            if k < NSB:
                ts0 = k * TS
                Jt, W0, Wt = Jr.next(), W0r.next(), Wr.next()
                J3 = Jt.ap.rearrange("p (t j) -> p t j", t=TS)
                W03 = W0.ap.rearrange("p (j t) -> p j t", t=TS)
                W3 = Wt.ap.rearrange("p (j t) -> p j t", t=TS)
                tt("dve", J3, iotab.ap.unsqueeze(1).to_broadcast([128, TS, 128]), trT.ap[:, 1, ts0:ts0 + TS].unsqueeze(2).to_broadcast([128, TS, 128]),
                   ALU.is_equal, [iotab.b, trT.b], [Jt.b])
                tt("dve", W03, io_jt[:, i0:i0 + 64, :], trT.ap[:, 0, ts0:ts0 + TS].unsqueeze(1).to_broadcast([128, 64, TS]), ALU.is_equal,
                   [iota_jt.b, trT.b], [W0.b])
                tt("dve", W3, W03, trT.ap[:, 2, ts0:ts0 + TS].unsqueeze(1).to_broadcast([128, 64, TS]), ALU.mult, [W0.b, trT.b], [Wt.b])
                ctx[k] = [Jt, Wt, J3, W3]
            k1 = k - 1
            if 0 <= k1 < NSB:
                Jt, Wt, J3, W3 = ctx[k1]
                bank = abanks.next()
                for tl in range(TS):
                    mm(bank.ap[:, tl * 64:(tl + 1) * 64], J3[:, tl, :], W3[:, :, tl], True, True, [Jt.b, Wt.b], [bank.b])
                ctx[k1].append(bank)
            k2 = k - 2
            if 0 <= k2 < NSB:
                bank = ctx[k2][4]
                ts0 = k2 * TS
                cp("act", GT.ap[:, i0:i0 + 64, ts0:ts0 + TS], bank.ap.rearrange("p (t i) -> p i t", t=TS), [bank.b], [b_GT[h]])
                del ctx[k2]
            yield

    def stage_C(p, gen1, gen2):
        r0 = p * TP
        x1t, h2T = x1ts[p % 2], h2Ts[p % 2]

        def emit_AT(i):
            uc, vc = ucr.next(), vcr.next()
            dma("sp", uc.ap, UT_s[i * 128:(i + 1) * 128, :], [b_UT[i // 4]], [uc.b])
            dma("sp", vc.ap, V_s[i * 128:(i + 1) * 128, :], [b_V[i // 4]], [vc.b])
            ab = cbanks.next()
            for dc in range(8):
                mm(ab.ap[:, 0:TP], uc.ap[:, dc * 128:(dc + 1) * 128], h2T.ap[:, dc, :], dc == 0, dc == 7, [uc.b, h2T.b], [ab.b])
            return ab, vc

        def emit_gelu(i, ab):
            ga = gar.next()
            act(ga.ap, ab.ap[:, 0:TP], AF.Gelu_apprx_tanh, [ab.b], [ga.b])
            return ga

        def emit_mult(i, ga):
            GA = GAr.next()
            tt("dve", GA.ap, ga.ap, GT.ap[:, i, :], ALU.mult, [ga.b, b_GT[i // 64]], [GA.b])
            return GA

        def emit_out(i, GA, vc):
            for tb in range(2):
                for half in range(2):
                    a_ = acc[tb * 2 + half]
                    mm(a_.ap, GA.ap[:, tb * 128:(tb + 1) * 128], vc.ap[:, half * 512:(half + 1) * 512], i == 0, i == 127, [GA.b, vc.b], [a_.b])

        st_ab, st_vc, st_ga, st_GA = {}, {}, {}, {}
        st_ab[0], st_vc[0] = emit_AT(0)
        for i in range(128 + 2):
            if i + 1 < 128:
                st_ab[i + 1], st_vc[i + 1] = emit_AT(i + 1)
            if i < 128:
                st_ga[i] = emit_gelu(i, st_ab.pop(i))
            if 0 <= i - 1 < 128:
                st_GA[i - 1] = emit_mult(i - 1, st_ga.pop(i - 1))
            if 0 <= i - 2 < 128:
                emit_out(i - 2, st_GA.pop(i - 2), st_vc.pop(i - 2))
            if gen1 is not None and next(gen1, "done") == "done":
                gen1 = None
                if i < 64:
                    continue
            if gen1 is None and i >= 64 and gen2 is not None:
                if next(gen2, "done") == "done":
                    gen2 = None
        for g_ in (gen1, gen2):
            if g_ is not None:
                for _ in g_:
                    pass
        for tb in range(2):
            xt_ = x1t[tb]
            for half in range(2):
                a_ = acc[tb * 2 + half]
                hs = slice(half * 512, (half + 1) * 512)
                tt("dve", xt_.ap[:, hs], xt_.ap[:, hs], a_.ap, ALU.add, [xt_.b, a_.b], [xt_.b])
            jk, st = xnr.next(), pst.next()
            act(jk.ap, xt_.ap, AF.Square, [xt_.b], [jk.b, st.b], accum=st.ap[:, 0:1])
            act(st.ap[:, 1:2], st.ap[:, 0:1], AF.Sqrt, [st.b, smalls.b], [st.b], bias=eps_t, scale=1.0 / DM)
            recip(st.ap[:, 2:3], st.ap[:, 1:2], [st.b], [st.b])
            stt(xt_.ap, xt_.ap, st.ap[:, 2:3], gfin.ap, ALU.mult, ALU.mult, [xt_.b, st.b, gfin.b], [xt_.b])
            dma("sp", out_d[r0 + tb * 128:r0 + (tb + 1) * 128, :], xt_.ap, [xt_.b], [])

    import itertools
    for _ in stage_A(0):
        pass
    for _ in stage_B(0, 0):
        pass
    for p in range(NP):
        if p + 1 < NP:
            stage_C(p, itertools.chain(stage_B(p, 1), stage_A(p + 1)), stage_B(p + 1, 0))
        else:
            stage_C(p, stage_B(p, 1), None)
    S.finish()
    S.replay()
    print("arena peak bytes", ar.peak * 4)
    return nc


def _layouts(inp):
    f = lambda a: np.ascontiguousarray(np.asarray(a, dtype=np.float32))
    w_in = f(inp["w_in"])[0]
    slab = lambda w, nk: np.ascontiguousarray(
        w.reshape(nk, 128, w.shape[1] // 128, 128).transpose(2, 1, 0, 3).reshape(w.shape[1] // 128, 128, nk * 128))
    d = {}
    d["w_in_h"] = slab(w_in, 8)
    d["w_mkv_h"] = slab(f(inp["w_mem_kv"])[0], 8)
    d["w_pq_h"] = slab(f(inp["w_peer_q"])[0], 8)
    d["p_attn_h"] = slab(f(inp["p_attn"])[0], 6)
    d["p_lru_h"] = slab(f(inp["p_lru"])[0], 6)
    d["p_mem_h"] = slab(f(inp["p_mem"])[0], 4)
    d["w_out_h"] = np.ascontiguousarray(f(inp["w_out"])[0].reshape(8, 128, 1024).transpose(1, 0, 2).reshape(128, 8192))
    sk = f(inp["peer_sub_keys"])[0]
    d["skT_h"] = np.ascontiguousarray(sk.reshape(16, 128, 128).transpose(2, 0, 1).reshape(128, 2048))
    wr = f(inp["lru_wr"])[0].reshape(12, 128, 128)
    wi = f(inp["lru_wi"])[0].reshape(12, 128, 128)
    d["lruw_h"] = np.ascontiguousarray(np.concatenate([wr, wi], 0).transpose(1, 0, 2).reshape(128, 3072))
    u = f(inp["peer_u"])[0]
    d["uT_h"] = np.ascontiguousarray(u.reshape(128, 128, 8, 128).transpose(0, 3, 2, 1).reshape(128 * 128, 1024))
    d["v_h"] = f(inp["peer_v"])[0]
    vec = np.zeros((128, 96), np.float32)
    vec[:, 0:24] = f(inp["conv_w"])[0].reshape(4, 6, 128).transpose(2, 1, 0).reshape(128, 24)
    vec[:, 24:30] = f(inp["conv_b"])[0].reshape(6, 128).T
    vec[:, 30:42] = f(inp["lru_br"])[0].reshape(12, 128).T
    vec[:, 42:54] = f(inp["lru_bi"])[0].reshape(12, 128).T
    vec[:, 54:66] = f(inp["lru_lambda"])[0].reshape(12, 128).T
    vec[:, 66:90] = f(inp["gate_b"])[0].reshape(24, 128).T
    inv_freq = (np.float32(500000.0) ** (-np.arange(0, 32, 2, dtype=np.float32) / np.float32(32))).astype(np.float32)
    vec[0:32, 90] = np.concatenate([inv_freq, inv_freq])
    d["vec_h"] = vec
    bc = np.stack([f(inp["g_mix"])[0], f(inp["g_mem"])[0], f(inp["g_ffn"])[0], f(inp["g_final"])], 0)
    d["bc_h"] = np.ascontiguousarray(np.broadcast_to(bc[None], (128, 4, 1024)))
    d["sink_h"] = np.ascontiguousarray(np.broadcast_to(f(inp["attn_sink"])[0][None], (128, 6)))
    c = np.zeros((128, 816), np.float32)
    c[:, 0:128] = np.eye(128, dtype=np.float32)
    for m in range(16):
        c[m + 16, 688 + m] = -1.0
        c[m, 688 + 16 + m] = 1.0
    p = np.arange(128)[:, None]
    j = np.arange(384)[None, :]
    c[:, 160:544] = np.where((j >= p) & (j <= p + 256), 0.0, -1e30)
    c[:, 544:672] = np.arange(128, dtype=np.float32)[None, :]
    c[:, 672:688] = np.arange(16, dtype=np.float32)[None, :]
    d["consts_h"] = c
    return d


_NC_CACHE = {}


def kernel(**inputs):
    shared = _layouts(inputs)
    x = np.ascontiguousarray(np.asarray(inputs["x"], dtype=np.float32))
    mem = np.ascontiguousarray(np.asarray(inputs["mem"], dtype=np.float32))
    pos = np.ascontiguousarray(np.asarray(inputs["positions"]).astype(np.int32))
    in_maps = []
    for c in range(N_CORES):
        m = dict(shared)
        sl = slice(c * NSEQ_CORE, (c + 1) * NSEQ_CORE)
        m["x"] = x[sl]
        m["mem"] = mem[sl]
        m["pos"] = np.ascontiguousarray(np.broadcast_to(pos[sl][:, None, :], (NSEQ_CORE, 128, SEQ)))
        in_maps.append(m)
    nc = build_program()
    res = run_bass_kernel_spmd(nc, in_maps, core_ids=list(range(N_CORES)))
    outs = [np.asarray(r["out"]).reshape(NSEQ_CORE, SEQ, DM) for r in res.results]
    return np.concatenate(outs, 0).astype(np.float32)
```

```python
import numpy as np
import concourse.bass as bass
import concourse.mybir as mybir
from concourse.bass_utils import run_bass_kernel_spmd

F32 = mybir.dt.float32
BF16 = mybir.dt.bfloat16
I32 = mybir.dt.int32
U32 = mybir.dt.uint32
AF = mybir.ActivationFunctionType
ALU = mybir.AluOpType
AX = mybir.AxisListType

N_CORES = 8
SEQ = 2048
DM = 1024
NSEQ_CORE = 2
OQ, OKK, OV, OXL, OGL, OQM, OG = 0, 768, 1024, 1280, 2048, 2816, 3328
EPS = 1e-6
TP = 256


class Buf:
    __slots__ = ("name", "w", "r", "excl")

    def __init__(self, name="", excl=False):
        self.name = name
        self.w = None
        self.r = []
        self.excl = excl


class Ins:
    __slots__ = ("eng", "seq", "fn", "signal", "cnt", "dma", "sem", "target", "waits")


class Sched:
    ENGS = ["pe", "act", "dve", "pool", "sp"]

    def __init__(self, nc, n_dma_sems=16, n_pool_sems=3):
        self.nc = nc
        self.prog = {e: [] for e in self.ENGS}
        self.known = {e: {} for e in self.ENGS}
        self.sem = {e: nc.alloc_semaphore("s_" + e) for e in self.ENGS}
        nq = {"sp": n_dma_sems, "pool": n_pool_sems}
        self.dsem = {q: [nc.alloc_semaphore(f"d_{q}{i}") for i in range(nq[q])] for q in ("sp", "pool")}
        self.dcount = {q: [0] * nq[q] for q in ("sp", "pool")}
        self.dlast = {q: [None] * nq[q] for q in ("sp", "pool")}
        self.drr = {"sp": 0, "pool": 0}

    def _need(self, eng, dep, waits):
        if dep is None:
            return
        if dep.dma:
            key = ("d", id(dep.sem))
            if self.known[eng].get(key, 0) >= dep.target:
                return
            self.known[eng][key] = dep.target
            waits.append(dep)
        else:
            if dep.eng == "pe" and eng == "pe":
                return
            key = dep.eng
            if self.known[eng].get(key, -1) >= dep.seq:
                return
            self.known[eng][key] = dep.seq
            dep.signal = True
            waits.append(dep)

    def _deps(self, eng, reads, writes):
        cands = []
        for b in reads:
            if b.w is not None:
                cands.append(b.w)
            if b.excl:
                cands.extend(r for r in b.r if r.eng != eng)
        for b in writes:
            if b.w is not None:
                cands.append(b.w)
            cands.extend(b.r)
        best = {}
        for d in cands:
            key = ("d", id(d.sem)) if d.dma else d.eng
            val = d.target if d.dma else d.seq
            if key not in best or val > best[key][0]:
                best[key] = (val, d)
        waits = []
        for key in best:
            self._need(eng, best[key][1], waits)
        return waits

    def _commit(self, ins, reads, writes):
        for b in reads:
            b.r.append(ins)
        for b in writes:
            b.w = ins
            b.r = []

    def _new(self, eng, fn):
        ins = Ins()
        ins.eng = eng
        ins.fn = fn
        ins.dma = False
        ins.signal = False
        ins.cnt = None
        ins.waits = []
        return ins

    def op(self, eng, fn, reads=(), writes=()):
        ins = self._new(eng, fn)
        ins.waits = self._deps(eng, reads, writes)
        ins.seq = len(self.prog[eng])
        self.prog[eng].append(ins)
        self._commit(ins, reads, writes)
        return ins

    def dma(self, q, fn, reads=(), writes=()):
        ins = self._new(q, fn)
        ins.dma = True
        ins.signal = True
        k = self.drr[q]
        self.drr[q] = (k + 1) % len(self.dsem[q])
        ins.sem = self.dsem[q][k]
        self.dcount[q][k] += 1
        ins.target = 16 * self.dcount[q][k]
        ins.waits = self._deps(q, reads, writes)
        prev = self.dlast[q][k]
        if prev is not None:
            self._need(q, prev, ins.waits)
        self.dlast[q][k] = ins
        ins.seq = len(self.prog[q])
        self.prog[q].append(ins)
        self._commit(ins, reads, writes)
        return ins

    def _lasts(self):
        lasts = []
        for e in self.ENGS:
            for i in reversed(self.prog[e]):
                if (not i.dma) and i.fn is not None:
                    lasts.append(i)
                    break
        for q in ("sp", "pool"):
            for i in self.dlast[q]:
                if i is not None:
                    lasts.append(i)
        return lasts

    def barrier(self, engines=None):
        lasts = self._lasts()
        for e in engines or self.ENGS:
            waits = []
            for d in lasts:
                if (not d.dma) and d.eng == e:
                    continue
                self._need(e, d, waits)
            if waits:
                ins = self._new(e, None)
                ins.waits = waits
                ins.seq = len(self.prog[e])
                self.prog[e].append(ins)

    def finish(self):
        self.barrier(engines=["sp"])

    def replay(self):
        nc = self.nc
        for e in self.ENGS:
            c = 0
            for i in self.prog[e]:
                if i.dma or i.fn is None:
                    continue
                if i.signal:
                    c += 1
                    i.cnt = c
        with nc.Block() as block:
            def run(engname, handle):
                for i in self.prog[engname]:
                    for d in i.waits:
                        if d.dma:
                            handle.wait_ge(d.sem, d.target)
                        else:
                            handle.wait_ge(self.sem[d.eng], d.cnt)
                    if i.fn is None:
                        continue
                    r = i.fn(handle)
                    if i.dma:
                        r.then_inc(i.sem, 16)
                    elif i.signal:
                        r.then_inc(self.sem[engname], 1)

            @block.tensor
            def _(t):
                run("pe", t)

            @block.scalar
            def _(s):
                run("act", s)

            @block.vector
            def _(v):
                run("dve", v)

            @block.gpsimd
            def _(g):
                run("pool", g)

            @block.sync
            def _(s):
                run("sp", s)


class Arena:
    ESZ = {F32: 4, BF16: 2, I32: 4, U32: 4}

    def __init__(self, ap, nwords):
        self.ap = ap
        self.n = nwords
        self.top = 0
        self.peak = 0

    def alloc(self, free_shape, dt, parts=128):
        n = int(np.prod(free_shape))
        nw = (n * self.ESZ[dt] + 3) // 4
        nw8 = (nw + 7) // 8 * 8
        o = self.top
        self.top += nw8
        self.peak = max(self.peak, self.top)
        assert self.top <= self.n, f"arena overflow {self.top*4} > {self.n*4}"
        v = self.ap[0:parts, o:o + nw]
        if dt != F32:
            v = v.bitcast(dt)
        if len(free_shape) == 2:
            v = v.rearrange("p (a b) -> p a b", a=free_shape[0])
        elif len(free_shape) == 3:
            v = v.rearrange("p (a b c) -> p a b c", a=free_shape[0], b=free_shape[1])
        return v

    def mark(self):
        return self.top

    def release(self, m):
        self.top = m


class Ring:
    def __init__(self, items):
        self.items = items
        self.i = 0

    def next(self):
        it = self.items[self.i]
        self.i = (self.i + 1) % len(self.items)
        return it


class TB:
    __slots__ = ("ap", "b")

    def __init__(self, ap, name="", excl=False):
        self.ap = ap
        self.b = Buf(name, excl)


def build_program(NSEQ=NSEQ_CORE, stop_after=None):
    nc = bass.Bass("TRN2", target_bir_lowering=False)
    NTOK = NSEQ * SEQ

    def din(name, shape, dt=F32):
        return nc.dram_tensor(name, list(shape), dt, kind="ExternalInput").ap()

    def dscr(name, shape, dt):
        return nc.dram_tensor(name, list(shape), dt).ap()

    x_d = din("x", [NSEQ, SEQ, DM])
    mem_d = din("mem", [NSEQ, 256, DM])
    pos_d = din("pos", [NSEQ, 128, SEQ], I32)
    win_h = din("w_in_h", [50, 128, 1024])
    wmkv_h = din("w_mkv_h", [8, 128, 1024])
    wpq_h = din("w_pq_h", [16, 128, 1024])
    pattn_h = din("p_attn_h", [8, 128, 768])
    plru_h = din("p_lru_h", [8, 128, 768])
    pmem_h = din("p_mem_h", [8, 128, 512])
    wout_h = din("w_out_h", [128, 8192])
    skT_h = din("skT_h", [128, 2048])
    lruw_h = din("lruw_h", [128, 3072])
    uT_h = din("uT_h", [128 * 128, 1024])
    v_h = din("v_h", [128 * 128, 1024])
    vec_h = din("vec_h", [128, 96])
    bc_h = din("bc_h", [128, 4, 1024])
    sink_h = din("sink_h", [128, 6])
    consts_h = din("consts_h", [128, 816])
    if stop_after in (None, "P1"):
        out_d = nc.dram_tensor("out", [NTOK, DM], F32, kind="ExternalOutput").ap()
        X1 = dscr("X1", [NTOK, DM], F32)
    else:
        X1 = nc.dram_tensor("out", [NTOK, DM], F32, kind="ExternalOutput").ap()
        out_d = None

    Win_s = dscr("Win_s", [50, 128, 1024], BF16)
    Wmkv_s = dscr("Wmkv_s", [8, 128, 1024], BF16)
    Wpq_s = dscr("Wpq_s", [16, 128, 1024], BF16)
    Pattn_s = dscr("Pattn_s", [8, 128, 768], BF16)
    Plru_s = dscr("Plru_s", [8, 128, 768], BF16)
    Pmem_s = dscr("Pmem_s", [8, 128, 512], BF16)
    UT_s = dscr("UT_s", [128 * 128, 1024], BF16)
    V_s = dscr("V_s", [128 * 128, 1024], BF16)

    S = Sched(nc)
    b_X1 = [Buf(f"X1_{i}") for i in range(NTOK // 128)]
    AW = 52992
    arena_t = nc.alloc_sbuf_tensor("arena", [128, AW], F32)
    ar = Arena(arena_t[:], AW)
    PS = [TB(nc.alloc_psum_tensor(f"ps{i}", [128, 512], F32)[:], f"ps{i}", True) for i in range(8)]
    PF = PS[0:6]
    PB = []
    for i in (6, 7):
        t_ = TB(PS[i].ap.bitcast(BF16), f"pb{i}")
        t_.b = PS[i].b
        PB.append(t_)
    pbr = Ring(PB)
    pfr = Ring(PF)

    def mm(out, lhsT, rhs, start, stop, r, w):
        S.op("pe", lambda e: e.matmul(out, lhsT=lhsT, rhs=rhs, start=start, stop=stop), r, w)

    def tr(out, in_, ident, r, w):
        S.op("pe", lambda e: e.transpose(out=out, in_=in_, identity=ident), r, w)

    def act(out, in_, func, r, w, bias=None, scale=1.0, accum=None):
        kw = {}
        if bias is not None:
            kw["bias"] = bias
        if accum is not None:
            kw["accum_out"] = accum
        S.op("act", lambda e: e.activation(out=out, in_=in_, func=func, scale=scale, **kw), r, w)

    def cp(eng, out, in_, r, w):
        if eng == "act":
            S.op("act", lambda e: e.copy(out=out, in_=in_), r, w)
        else:
            S.op(eng, lambda e: e.tensor_copy(out=out, in_=in_), r, w)

    def tt(eng, out, in0, in1, op, r, w):
        S.op(eng, lambda e: e.tensor_tensor(out=out, in0=in0, in1=in1, op=op), r, w)

    def ts(eng, out, in0, s1, s2, op0, op1, r, w):
        if s2 is None:
            S.op(eng, lambda e: e.tensor_scalar(out=out, in0=in0, scalar1=s1, scalar2=None, op0=op0), r, w)
        else:
            S.op(eng, lambda e: e.tensor_scalar(out=out, in0=in0, scalar1=s1, scalar2=s2, op0=op0, op1=op1), r, w)

    def stt(out, in0, scalar, in1, op0, op1, r, w):
        S.op("dve", lambda e: e.scalar_tensor_tensor(out=out, in0=in0, scalar=scalar, in1=in1, op0=op0, op1=op1), r, w)

    def red(out, in_, op, r, w):
        S.op("dve", lambda e: e.tensor_reduce(out=out, in_=in_, axis=AX.X, op=op), r, w)

    def recip(out, in_, r, w):
        S.op("dve", lambda e: e.reciprocal(out=out, in_=in_), r, w)

    def dma(q, out, in_, r, w, **kw):
        S.dma(q, lambda e: e.dma_start(out=out, in_=in_, **kw), r, w)

    def memset(eng, ap, val, w):
        S.op(eng, lambda e: e.memset(ap, val), (), w)

    b_Win = [Buf() for _ in range(50)]
    b_Wmkv, b_Wpq, b_Pattn, b_Plru, b_Pmem = Buf(), Buf(), Buf(), Buf(), Buf()
    b_UT = [Buf() for _ in range(32)]
    b_V = [Buf() for _ in range(32)]
    CK = dict(max_dma_last_dim=4096)

    consts = TB(ar.alloc((816,), F32), "consts")
    dma("sp", consts.ap, consts_h, [], [consts.b])
    identf = consts.ap[:, 0:128]
    mask = consts.ap[:, 160:544]
    iota128 = consts.ap[:, 544:672]
    iota16 = consts.ap[:, 672:688]
    identb = TB(ar.alloc((128,), BF16), "identb")
    dma("pool", identb.ap, consts_h[:, 0:128], [], [identb.b])
    permb = TB(ar.alloc((128,), BF16), "permb")
    dma("pool", permb.ap, consts_h[:, 688:816], [], [permb.b])
    maskb = TB(ar.alloc((384,), BF16), "maskb")
    dma("pool", maskb.ap, consts_h[:, 160:544], [], [maskb.b])
    vec = TB(ar.alloc((96,), F32), "vec")
    dma("sp", vec.ap, vec_h, [], [vec.b])
    sinkb = TB(ar.alloc((8,), F32), "sinkb")
    dma("sp", sinkb.ap[:, 0:6], sink_h, [], [sinkb.b])
    smalls = TB(ar.alloc((8,), F32), "smalls")
    memset("pool", smalls.ap[:, 0:1], EPS, [smalls.b])
    memset("pool", smalls.ap[:, 1:2], 1.0, [smalls.b])
    eps_t = smalls.ap[:, 0:1]
    one_t = smalls.ap[:, 1:2]
    lamv = TB(ar.alloc((16,), F32), "lamv")
    CW, CB, BR, BI, LAM, GB, INVF = 0, 24, 30, 42, 54, 66, 90

    def conv_win(g):
        dma("pool", Win_s[g * 5:(g + 1) * 5].rearrange("a p f -> (a p) f"),
            win_h[g * 5:(g + 1) * 5].rearrange("a p f -> (a p) f"), [], [b_Win[g * 5 + k] for k in range(5)], **CK)

    dma("pool", Wmkv_s.rearrange("a p f -> (a p) f"), wmkv_h.rearrange("a p f -> (a p) f"), [], [b_Wmkv], **CK)
    for g in (2, 3, 4, 0, 1):
        conv_win(g)

    def late_conv():
        for g in (5, 6, 7, 8, 9):
            conv_win(g)
        late_conv_rest()

    def late_conv_rest():
        dma("pool", Pattn_s.rearrange("a p f -> (a p) f"), pattn_h.rearrange("a p f -> (a p) f"), [], [b_Pattn], **CK)
        dma("pool", Plru_s.rearrange("a p f -> (a p) f"), plru_h.rearrange("a p f -> (a p) f"), [], [b_Plru], **CK)
        dma("pool", Pmem_s.rearrange("a p f -> (a p) f"), pmem_h.rearrange("a p f -> (a p) f"), [], [b_Pmem], **CK)
        for g in range(2):
            dma("pool", Wpq_s[g * 8:(g + 1) * 8].rearrange("a p f -> (a p) f"),
                wpq_h[g * 8:(g + 1) * 8].rearrange("a p f -> (a p) f"), [], [b_Wpq], **CK)

    def table_conv():
        for g in range(32):
            dma("pool", UT_s[g * 512:(g + 1) * 512, :], uT_h[g * 512:(g + 1) * 512, :], [], [b_UT[g]], **CK)
            yield
            dma("pool", V_s[g * 512:(g + 1) * 512, :], v_h[g * 512:(g + 1) * 512, :], [], [b_V[g]], **CK)
            yield

    tconv = table_conv()

    def conv_some(n):
        if stop_after not in (None, "P1"):
            return
        for _ in range(n):
            try:
                next(tconv)
            except StopIteration:
                return

    mtmp = ar.mark()
    t_e = TB(ar.alloc((16,), F32)); t_u = TB(ar.alloc((16,), F32)); t_l = TB(ar.alloc((16,), F32))
    lam_ap = vec.ap[:, LAM:LAM + 12]
    act(t_e.ap[:, 0:12], lam_ap, AF.Exp, [vec.b], [t_e.b], scale=-1.0)
    ts("dve", t_u.ap[:, 0:12], t_e.ap[:, 0:12], 1.0, None, ALU.add, None, [t_e.b], [t_u.b])
    act(t_l.ap[:, 0:12], t_u.ap[:, 0:12], AF.Ln, [t_u.b], [t_l.b])
    ts("dve", t_u.ap[:, 0:12], t_u.ap[:, 0:12], -1.0, 1e-30, ALU.add, ALU.max, [t_u.b], [t_u.b])
    recip(t_u.ap[:, 0:12], t_u.ap[:, 0:12], [t_u.b], [t_u.b])
    tt("dve", t_l.ap[:, 0:12], t_l.ap[:, 0:12], t_u.ap[:, 0:12], ALU.mult, [t_l.b, t_u.b], [t_l.b])
    tt("dve", t_l.ap[:, 0:12], t_l.ap[:, 0:12], t_e.ap[:, 0:12], ALU.mult, [t_l.b, t_e.b], [t_l.b])
    ts("dve", lamv.ap[:, 0:12], t_l.ap[:, 0:12], -8.0, None, ALU.mult, None, [t_l.b], [lamv.b])

    SCALE = float(128 ** -0.5)
    m_phase = ar.mark()

    def early_exit():
        S.finish()
        S.replay()
        return nc

    if stop_after == "P0":
        return early_exit()

    hT = ar.alloc((8, SEQ), BF16)
    b_hT = [Buf(f"hT{i}") for i in range(4)]
    kT = TB(ar.alloc((2, SEQ), BF16), "kT")
    vtok = TB(ar.alloc((16, 256), BF16), "vtok")
    lruT = ar.alloc((6, SEQ), BF16)
    b_lruT = [Buf(f"lruT{i}") for i in range(6)]
    Ct = TB(ar.alloc((SEQ,), F32), "C")
    St = TB(ar.alloc((SEQ,), F32), "S")
    kmT = TB(ar.alloc((4, 256), BF16), "kmT")
    vm = TB(ar.alloc((2, 512), BF16), "vm")
    wout = TB(ar.alloc((8, 1024), BF16), "wout")
    slabs = Ring([TB(ar.alloc((1024,), BF16), f"slab{i}") for i in range(8)])
    dma("pool", wout.ap.rearrange("p a b -> p (a b)"), wout_h, [], [wout.b], **CK)
    r_mark = ar.mark()

    def load_slab(src_ap, src_bufs, n=1024):
        sl = slabs.next()
        dma("sp", sl.ap[:, 0:n], src_ap, src_bufs, [sl.b])
        return sl

    def proj_fm(fc_src_ap, src_bufs, rhs_fn, rhs_bufs, ncols, nk=8):
        sl = load_slab(fc_src_ap, src_bufs, nk * 128)
        bank = pfr.next()
        for kc in range(nk):
            mm(bank.ap[:, 0:ncols], sl.ap[:, kc * 128:(kc + 1) * 128], rhs_fn(kc), kc == 0, kc == nk - 1,
               [sl.b] + rhs_bufs, [bank.b])
        return bank

    def rmsnorm_T(src_rows_ap, gb_ap, gb_buf, xt, xn, stat, dstT_ap, dst_bufs, cp_eng, src_bufs=()):
        dma("sp", xt.ap, src_rows_ap, list(src_bufs), [xt.b])
        act(xn.ap, xt.ap, AF.Square, [xt.b], [xn.b, stat.b], accum=stat.ap[:, 0:1])
        act(stat.ap[:, 1:2], stat.ap[:, 0:1], AF.Sqrt, [stat.b, smalls.b], [stat.b], bias=eps_t, scale=1.0 / DM)
        recip(stat.ap[:, 2:3], stat.ap[:, 1:2], [stat.b], [stat.b])
        stt(xn.ap, xt.ap, stat.ap[:, 2:3], gb_ap, ALU.mult, ALU.mult, [xt.b, stat.b, gb_buf], [xn.b])
        pb = pbr.next()
        for dc in range(8):
            tr(pb.ap[:, dc * 128:(dc + 1) * 128], xn.ap[:, dc * 128:(dc + 1) * 128], identb.ap, [xn.b, identb.b], [pb.b])
        cp(cp_eng, dstT_ap, pb.ap.rearrange("p (a b) -> p a b", a=8), [pb.b], dst_bufs)

    def range_reduce_sin(out_tb, ang_tb, tmpf, tmpi, n, parts=128):
        TWO_PI = float(2 * np.pi)
        a = ang_tb.ap[0:parts, 0:n]
        kf = tmpf.ap[0:parts, 0:n]
        ki = tmpi.ap[0:parts, 0:n]
        ts("dve", kf, a, 1.0 / TWO_PI, None, ALU.mult, None, [ang_tb.b], [tmpf.b])
        cp("dve", ki, kf, [tmpf.b], [tmpi.b])
        cp("dve", kf, ki, [tmpi.b], [tmpf.b])
        stt(a, kf, -TWO_PI, a, ALU.mult, ALU.add, [tmpf.b, ang_tb.b], [ang_tb.b])
        ts("dve", kf, a, float(np.pi), -TWO_PI, ALU.is_gt, ALU.mult, [ang_tb.b], [tmpf.b])
        tt("dve", a, a, kf, ALU.add, [ang_tb.b, tmpf.b], [ang_tb.b])
        ts("dve", kf, a, float(-np.pi), TWO_PI, ALU.is_lt, ALU.mult, [ang_tb.b], [tmpf.b])
        tt("dve", a, a, kf, ALU.add, [ang_tb.b, tmpf.b], [ang_tb.b])
        act(out_tb.ap[0:parts, 0:n], a, AF.Sin, [ang_tb.b], [out_tb.b])

    def rope_evac(bank, dst_ap, dst_bufs, tok0, n, tmpb, tmpa, tmpc):
        import os
        nst = int(os.environ.get("ROPE_STEPS", "9"))
        cp("act", tmpb.ap[:, 0:n], bank.ap[:, 0:n], [bank.b], [tmpb.b])
        if nst < 2: return
        pbk = pfr.next()
        mm(pbk.ap[:, 0:n], permb.ap, tmpb.ap[:, 0:n], True, True, [permb.b, tmpb.b], [pbk.b])
        if nst < 3: return
        tt("dve", tmpa.ap[:, 0:n], bank.ap[:, 0:n], Ct.ap[:, tok0:tok0 + n], ALU.mult, [bank.b, Ct.b], [tmpa.b])
        if nst < 4: return
        tt("dve", tmpc.ap[:, 0:n], pbk.ap[:, 0:n], St.ap[:, tok0:tok0 + n], ALU.mult, [pbk.b, St.b], [tmpc.b])
        if nst < 5: return
        tt("dve", dst_ap, tmpa.ap[:, 0:n], tmpc.ap[:, 0:n], ALU.add, [tmpa.b, tmpc.b], dst_bufs)

    for s in range(NSEQ):
        if s > 0:
            S.barrier()
        ar.release(r_mark)
        gmix = TB(ar.alloc((1024,), F32), "gmix")
        gmem = TB(ar.alloc((1024,), F32), "gmem")
        dma("sp", gmix.ap, bc_h[:, 0, :], [], [gmix.b])
        dma("sp", gmem.ap, bc_h[:, 1, :], [], [gmem.b])
        xts = [TB(ar.alloc((1024,), F32), f"xt{i}") for i in range(4)]
        xns = [TB(ar.alloc((1024,), BF16), f"xn{i}") for i in range(4)]
        stats = [TB(ar.alloc((8,), F32), f"st{i}") for i in range(4)]
        memT = TB(ar.alloc((8, 256), BF16), "memT")
        posi = TB(ar.alloc((SEQ,), I32), "posi")
        ang = TB(ar.alloc((SEQ,), F32), "ang")
        ang2 = TB(ar.alloc((SEQ,), F32), "ang2")
        tmpf = TB(ar.alloc((SEQ,), F32), "tmpf")
        tmpi = TB(ar.alloc((SEQ,), I32), "tmpi")
        for tb in range(16):
            k = tb % 4
            rmsnorm_T(x_d[s, tb * 128:(tb + 1) * 128, :], gmix.ap, gmix.b, xts[k], xns[k], stats[k],
                      hT[:, :, tb * 128:(tb + 1) * 128], [b_hT[tb // 4]], "act" if k else "dve")
            if stop_after == "M0a":
                return early_exit()
        if stop_after == "M0b":
            return early_exit()
        for mb in range(2):
            rmsnorm_T(mem_d[s, mb * 128:(mb + 1) * 128, :], gmem.ap, gmem.b, xts[mb], xns[mb], stats[mb],
                      memT.ap[:, :, mb * 128:(mb + 1) * 128], [memT.b], "act" if mb else "dve")
        dma("sp", posi.ap, pos_d[s], [], [posi.b])
        cp("dve", ang.ap, posi.ap, [posi.b], [ang.b])
        ts("dve", ang.ap, ang.ap, vec.ap[:, INVF:INVF + 1], None, ALU.mult, None, [ang.b, vec.b], [ang.b])
        ts("dve", ang2.ap, ang.ap, float(np.pi / 2), None, ALU.add, None, [ang.b], [ang2.b])
        range_reduce_sin(St, ang, tmpf, tmpi, SEQ)
        range_reduce_sin(Ct, ang2, tmpf, tmpi, SEQ)
        if stop_after == "M0c":
            return early_exit()
        for hd in range(4):
            bank = proj_fm(Wmkv_s[hd], [b_Wmkv], lambda kc: memT.ap[:, kc, :], [memT.b], 256)
            cp("act", kmT.ap[:, hd, :], bank.ap[:, 0:256], [bank.b], [kmT.b])
            if stop_after == "M0d":
                return early_exit()
        if stop_after == "M0e":
            return early_exit()
        for hd in range(4):
            sl = load_slab(Wmkv_s[4 + hd], [b_Wmkv])
            for mb in range(2):
                bank = pfr.next()
                for kc in range(8):
                    mm(bank.ap[:, 0:128], memT.ap[:, kc, mb * 128:(mb + 1) * 128], sl.ap[:, kc * 128:(kc + 1) * 128],
                       kc == 0, kc == 7, [memT.b, sl.b], [bank.b])
                cp("dve", vm.ap[:, mb, hd * 128:(hd + 1) * 128], bank.ap[:, 0:128], [bank.b], [vm.b])
        conv_some(4 if s else 0)
        if stop_after == "M0":
            return early_exit()

        S.barrier()
        ar.release(r_mark)
        lruw = TB(ar.alloc((24, 128), BF16), "lruw")
        dma("pool", lruw.ap.rearrange("p a b -> p (a b)"), lruw_h, [], [lruw.b], **CK)
        if s == 0:
            late_conv()
        T = [TB(ar.alloc((SEQ + 8,), F32), f"T{i}") for i in range(6)]
        xcb = TB(ar.alloc((SEQ,), BF16), "xcb")
        for cb in range(6):
            xl, xc, t2, t3, t4, t5 = T
            memset("pool", xl.ap[:, 0:2], 0.0, [xl.b])
            memset("pool", xl.ap[:, SEQ + 2:SEQ + 3], 0.0, [xl.b])
            sl = load_slab(Win_s[OXL // 128 + cb], [b_Win[OXL // 128 + cb]])
            for tc in range(4):
                bank = pfr.next()
                for kc in range(8):
                    mm(bank.ap, sl.ap[:, kc * 128:(kc + 1) * 128], hT[:, kc, tc * 512:(tc + 1) * 512], kc == 0, kc == 7,
                       [sl.b, b_hT[tc]], [bank.b])
                cp("act" if tc % 2 else "dve", xl.ap[:, 2 + tc * 512:2 + (tc + 1) * 512], bank.ap, [bank.b], [xl.b])
            cw = lambda j: vec.ap[:, CW + cb * 4 + j:CW + cb * 4 + j + 1]
            ts("dve", xc.ap[:, 0:SEQ], xl.ap[:, 0:SEQ], cw(0), vec.ap[:, CB + cb:CB + cb + 1], ALU.mult, ALU.add,
               [xl.b, vec.b], [xc.b])
            for j in range(1, 4):
                stt(xc.ap[:, 0:SEQ], xl.ap[:, j:j + SEQ], cw(j), xc.ap[:, 0:SEQ], ALU.mult, ALU.add, [xl.b, vec.b, xc.b], [xc.b])
            cp("act", xcb.ap, xc.ap[:, 0:SEQ], [xc.b], [xcb.b])
            for d in range(2):
                col = d * 6 + cb
                for (which, dst, bo) in ((0, t2, BR), (1, t3, BI)):
                    for tc in range(4):
                        bank = pfr.next()
                        mm(bank.ap, lruw.ap[:, which * 12 + col, :], xcb.ap[:, tc * 512:(tc + 1) * 512], True, True,
                           [lruw.b, xcb.b], [bank.b])
                        act(dst.ap[:, tc * 512:(tc + 1) * 512], bank.ap, AF.Sigmoid, [bank.b, vec.b], [dst.b],
                            bias=vec.ap[:, bo + col:bo + col + 1])
                a_ap = t2.ap[:, 0:SEQ]
                act(a_ap, a_ap, AF.Exp, [t2.b, lamv.b], [t2.b], scale=lamv.ap[:, col:col + 1])
                stt(t4.ap[:, 0:SEQ], a_ap, -1.0, a_ap, ALU.mult, ALU.mult, [t2.b], [t4.b])
                act(t4.ap[:, 0:SEQ], t4.ap[:, 0:SEQ], AF.Sqrt, [t4.b, smalls.b], [t4.b], bias=one_t)
                tt("dve", t3.ap[:, 0:SEQ], t3.ap[:, 0:SEQ], t4.ap[:, 0:SEQ], ALU.mult, [t3.b, t4.b], [t3.b])
                tt("dve", t3.ap[:, 0:SEQ], t3.ap[:, 0:SEQ], xc.ap[:, 0:SEQ], ALU.mult, [t3.b, xc.b], [t3.b])
                if d == 0:
                    S.op("dve", lambda e, o=t5.ap[:, 0:SEQ], a0=a_ap, b0=t3.ap[:, 0:SEQ]: e.tensor_tensor_scan(
                        out=o, data0=a0, data1=b0, initial=0.0, op0=ALU.mult, op1=ALU.add), [t2.b, t3.b], [t5.b])
                else:
                    S.op("dve", lambda e, o=t4.ap[:, SEQ - 1::-1], a0=t2.ap[:, SEQ - 1::-1], b0=t3.ap[:, SEQ - 1::-1]:
                         e.tensor_tensor_scan(out=o, data0=a0, data1=b0, initial=0.0, op0=ALU.mult, op1=ALU.add),
                         [t2.b, t3.b], [t4.b])
            tt("dve", t5.ap[:, 0:SEQ], t5.ap[:, 0:SEQ], t4.ap[:, 0:SEQ], ALU.add, [t5.b, t4.b], [t5.b])
            sl = load_slab(Win_s[OGL // 128 + cb], [b_Win[OGL // 128 + cb]])
            for tc in range(4):
                bank = pfr.next()
                for kc in range(8):
                    mm(bank.ap, sl.ap[:, kc * 128:(kc + 1) * 128], hT[:, kc, tc * 512:(tc + 1) * 512], kc == 0, kc == 7,
                       [sl.b, b_hT[tc]], [bank.b])
                act(xl.ap[:, tc * 512:(tc + 1) * 512], bank.ap, AF.Gelu_apprx_tanh, [bank.b], [xl.b])
            tt("dve", lruT[:, cb, :], xl.ap[:, 0:SEQ], t5.ap[:, 0:SEQ], ALU.mult, [xl.b, t5.b], [b_lruT[cb]])
            conv_some(2)

        if stop_after == "M1":
            return early_exit()
        S.barrier()
        ar.release(r_mark)
        qT = TB(ar.alloc((6, 512), BF16), "qT")
        qmT = TB(ar.alloc((4, 512), BF16), "qmT")
        aoT = TB(ar.alloc((6, 512), BF16), "aoT")
        moT = TB(ar.alloc((4, 512), BF16), "moT")
        mgT = TB(ar.alloc((8, 512), BF16), "mgT")
        gts = Ring([TB(ar.alloc((512,), F32), f"g{i}") for i in range(2)])
        accm = TB(ar.alloc((512,), F32), "accm")
        tmpm = Ring([TB(ar.alloc((512,), F32), f"tmpm{i}") for i in range(1)])
        xres = Ring([TB(ar.alloc((1024,), F32), f"xres{i}") for i in range(4)])
        rtb = Ring([TB(ar.alloc((512,), BF16), f"rtb{i}") for i in range(2)])
        rta = Ring([TB(ar.alloc((512,), F32), f"rta{i}") for i in range(1)])
        rtc = Ring([TB(ar.alloc((512,), F32), f"rtc{i}") for i in range(1)])
        NH = 6
        s_sb = [TB(ar.alloc((384,), F32), f"s{i}") for i in range(NH)]
        Pn = [TB(ar.alloc((384,), BF16), f"Pn{i}") for i in range(NH)]
        PT = [TB(ar.alloc((384,), BF16), f"PT{i}") for i in range(NH)]
        sm = [TB(ar.alloc((8,), F32), f"sm{i}") for i in range(NH)]

        for kvh in range(2):
            for tc in range(4):
                bank = proj_fm(Win_s[OKK // 128 + kvh], [b_Win[OKK // 128 + kvh]],
                               lambda kc: hT[:, kc, tc * 512:(tc + 1) * 512], [b_hT[tc]], 512)
                if stop_after == "M2p":
                    return early_exit()
                rope_evac(bank, kT.ap[:, kvh, tc * 512:(tc + 1) * 512], [kT.b], tc * 512, 512, rtb.next(), rta.next(), rtc.next())
                if stop_after == "M2a":
                    return early_exit()
        if stop_after == "M2b":
            return early_exit()
        for kvh in range(2):
            sl = load_slab(Win_s[OV // 128 + kvh], [b_Win[OV // 128 + kvh]])
            for tb in range(16):
                bank = pfr.next()
                for kc in range(8):
                    mm(bank.ap[:, 0:128], hT[:, kc, tb * 128:(tb + 1) * 128], sl.ap[:, kc * 128:(kc + 1) * 128],
                       kc == 0, kc == 7, [b_hT[tb // 4], sl.b], [bank.b])
                cp("act" if tb % 2 else "dve", vtok.ap[:, tb, kvh * 128:(kvh + 1) * 128], bank.ap[:, 0:128], [bank.b], [vtok.b])
        conv_some(4)
        if stop_after == "M2":
            return early_exit()

        def softmax_block(nh, score_fn, NK, mask_ap, sink_col, v_fn, nkb, out_fn):
            banks = []
            for i in range(nh):
                bank = pfr.next()
                score_fn(i, bank, mask_ap is None)
                if mask_ap is not None:
                    mm(bank.ap[:, 0:NK], identb.ap, mask_ap, False, True, [identb.b, maskb.b], [bank.b])
                banks.append(bank)
            for i in range(nh):
                red(sm[i].ap[:, 0:1], banks[i].ap[:, 0:NK], ALU.max, [banks[i].b], [sm[i].b])
                if sink_col is not None:
                    ts("dve", sm[i].ap[:, 0:1], sm[i].ap[:, 0:1], SCALE, sinkb.ap[:, sink_col(i):sink_col(i) + 1], ALU.mult, ALU.max,
                       [sm[i].b, sinkb.b], [sm[i].b])
                    ts("dve", sm[i].ap[:, 1:2], sm[i].ap[:, 0:1], -1.0, None, ALU.mult, None, [sm[i].b], [sm[i].b])
                else:
                    ts("dve", sm[i].ap[:, 1:2], sm[i].ap[:, 0:1], -SCALE, None, ALU.mult, None, [sm[i].b], [sm[i].b])
            for i in range(nh):
                sa = s_sb[i].ap[:, 0:NK]
                act(sa, banks[i].ap[:, 0:NK], AF.Exp, [banks[i].b, sm[i].b], [s_sb[i].b, sm[i].b], bias=sm[i].ap[:, 1:2], scale=SCALE,
                    accum=sm[i].ap[:, 2:3])
                if sink_col is not None:
                    act(sm[i].ap[:, 3:4], sm[i].ap[:, 1:2], AF.Exp, [sm[i].b, sinkb.b], [sm[i].b],
                        bias=sinkb.ap[:, sink_col(i):sink_col(i) + 1])
            for i in range(nh):
                sa = s_sb[i].ap[:, 0:NK]
                if sink_col is not None:
                    tt("dve", sm[i].ap[:, 4:5], sm[i].ap[:, 2:3], sm[i].ap[:, 3:4], ALU.add, [sm[i].b], [sm[i].b])
                    recip(sm[i].ap[:, 5:6], sm[i].ap[:, 4:5], [sm[i].b], [sm[i].b])
                else:
                    recip(sm[i].ap[:, 5:6], sm[i].ap[:, 2:3], [sm[i].b], [sm[i].b])
                ts("dve", Pn[i].ap[:, 0:NK], sa, sm[i].ap[:, 5:6], None, ALU.mult, None, [s_sb[i].b, sm[i].b], [Pn[i].b])
            pbs = []
            for i in range(nh):
                pb = pbr.next()
                for j in range(nkb):
                    tr(pb.ap[:, j * 128:(j + 1) * 128], Pn[i].ap[:, j * 128:(j + 1) * 128], identb.ap, [Pn[i].b, identb.b], [pb.b])
                cp("act", PT[i].ap[:, 0:NK], pb.ap[:, 0:NK], [pb.b], [PT[i].b])
            for i in range(nh):
                ob = pfr.next()
                for j in range(nkb):
                    vap, vb = v_fn(i, j)
                    mm(ob.ap[:, 0:128], vap, PT[i].ap[:, j * 128:(j + 1) * 128], j == 0, j == nkb - 1, vb + [PT[i].b], [ob.b])
                dst, db = out_fn(i)
                cp("act", dst, ob.ap[:, 0:128], [ob.b], db)

        for tc in range(4):
            t0 = tc * 512
            hb = [b_hT[tc]]
            rhs_h = lambda kc: hT[:, kc, t0:t0 + 512]
            for h in range(6):
                bank = proj_fm(Win_s[OQ // 128 + h], [b_Win[OQ // 128 + h]], rhs_h, hb, 512)
                rope_evac(bank, qT.ap[:, h, :], [qT.b], t0, 512, rtb.next(), rta.next(), rtc.next())
            for hd in range(4):
                bank = proj_fm(Win_s[OQM // 128 + hd], [b_Win[OQM // 128 + hd]], rhs_h, hb, 512)
                cp("act", qmT.ap[:, hd, :], bank.ap, [bank.b], [qmT.b])
            for qb in range(4):
                n = tc * 4 + qb
                kb0, kb1 = max(0, n - 1), min(15, n + 1)
                nkb = kb1 - kb0 + 1
                NK = nkb * 128
                moff = 0 if n > 0 else 128
                qs = slice(qb * 128, (qb + 1) * 128)

                def score_a(i, bank, stop, qs=qs, kb0=kb0, NK=NK):
                    mm(bank.ap[:, 0:NK], qT.ap[:, i, qs], kT.ap[:, i // 3, kb0 * 128:kb0 * 128 + NK], True, stop,
                       [qT.b, kT.b], [bank.b])

                softmax_block(6, score_a, NK, maskb.ap[:, moff:moff + NK], lambda i: i,
                              lambda i, j, kb0=kb0: (vtok.ap[:, kb0 + j, (i // 3) * 128:(i // 3 + 1) * 128], [vtok.b]),
                              nkb, lambda i, qs=qs: (aoT.ap[:, i, qs], [aoT.b]))

                def score_m(i, bank, stop, qs=qs):
                    mm(bank.ap[:, 0:256], qmT.ap[:, i, qs], kmT.ap[:, i, :], True, stop, [qmT.b, kmT.b], [bank.b])

                softmax_block(4, score_m, 256, None, None,
                              lambda i, j: (vm.ap[:, j, i * 128:(i + 1) * 128], [vm.b]),
                              2, lambda i, qs=qs: (moT.ap[:, i, qs], [moT.b]))
            xrs = []
            for tb in range(4):
                xr = xres.next()
                dma("sp", xr.ap, x_d[s, t0 + tb * 128:t0 + (tb + 1) * 128, :], [], [xr.b])
                xrs.append(xr)
            for dmc in range(8):
                for b in range(3):
                    fc = OG // 128 + b * 8 + dmc
                    gbank = proj_fm(Win_s[fc], [b_Win[fc]], rhs_h, hb, 512)
                    gt = gts.next()
                    act(gt.ap, gbank.ap, AF.Sigmoid, [gbank.b, vec.b], [gt.b], bias=vec.ap[:, GB + b * 8 + dmc:GB + b * 8 + dmc + 1])
                    if b == 0:
                        pbank = proj_fm(Pattn_s[dmc], [b_Pattn], lambda kc: aoT.ap[:, kc, :], [aoT.b], 512, nk=6)
                    elif b == 1:
                        pbank = proj_fm(Plru_s[dmc], [b_Plru], lambda kc: lruT[:, kc, t0:t0 + 512], [b_lruT[kc2] for kc2 in range(6)], 512, nk=6)
                    else:
                        pbank = proj_fm(Pmem_s[dmc], [b_Pmem], lambda kc: moT.ap[:, kc, :], [moT.b], 512, nk=4)
                    if b == 0:
                        tt("dve", accm.ap, gt.ap, pbank.ap, ALU.mult, [gt.b, pbank.b], [accm.b])
                    elif b == 1:
                        tm = tmpm.next()
                        tt("dve", tm.ap, gt.ap, pbank.ap, ALU.mult, [gt.b, pbank.b], [tm.b])
                        tt("pool", accm.ap, accm.ap, tm.ap, ALU.add, [accm.b, tm.b], [accm.b])
                    else:
                        tm = tmpm.next()
                        tt("dve", tm.ap, gt.ap, pbank.ap, ALU.mult, [gt.b, pbank.b], [tm.b])
                        tt("pool", mgT.ap[:, dmc, :], accm.ap, tm.ap, ALU.add, [accm.b, tm.b], [mgT.b])
            for tb in range(4):
                row0 = s * SEQ + t0 + tb * 128
                xr = xrs[tb]
                for half in range(2):
                    bank = pfr.next()
                    for dmc in range(8):
                        mm(bank.ap, mgT.ap[:, dmc, tb * 128:(tb + 1) * 128], wout.ap[:, dmc, half * 512:(half + 1) * 512],
                           dmc == 0, dmc == 7, [mgT.b, wout.b], [bank.b])
                    tt("dve", xr.ap[:, half * 512:(half + 1) * 512], xr.ap[:, half * 512:(half + 1) * 512], bank.ap, ALU.add,
                       [xr.b, bank.b], [xr.b])
                dma("sp", X1[row0:row0 + 128, :], xr.ap, [xr.b], [b_X1[row0 // 128]])
            conv_some(6)

    if stop_after == "M":
        S.finish()
        S.replay()
        print("arena peak bytes", ar.peak * 4)
        return nc

    conv_some(1000)
    S.barrier()
    ar.release(m_phase)
    skT = TB(ar.alloc((16, 128), BF16), "skT")
    dma("pool", skT.ap.rearrange("p a b -> p (a b)"), skT_h, [], [skT.b], **CK)
    gffn = TB(ar.alloc((1024,), F32), "gffn")
    gfin = TB(ar.alloc((1024,), F32), "gfin")
    dma("sp", gffn.ap, bc_h[:, 2, :], [], [gffn.b])
    dma("sp", gfin.ap, bc_h[:, 3, :], [], [gfin.b])
    GT = TB(ar.alloc((128, TP), BF16), "GT")
    h2Ts = [TB(ar.alloc((8, TP), BF16), f"h2T{i}") for i in range(2)]
    x1ts = [[TB(ar.alloc((1024,), F32), f"x1t{k}{i}") for i in range(2)] for k in range(2)]
    trTs = [TB(ar.alloc((3, TP), BF16), f"trT{i}") for i in range(2)]
    iotab = TB(ar.alloc((128,), BF16), "iotab")
    dma("pool", iotab.ap, consts_h[:, 544:672], [], [iotab.b])
    xnr = Ring([TB(ar.alloc((1024,), BF16), f"xnp{i}") for i in range(2)])
    pst = Ring([TB(ar.alloc((8,), F32), f"pst{i}") for i in range(4)])
    qpT = TB(ar.alloc((16, TP), BF16), "qpT")
    sc = TB(ar.alloc((16, 128), F32), "sc")
    wk = ar.alloc((16, 128), F32)
    b_wk = [Buf(f"wk{i}") for i in range(16)]
    cand_ap = sc.ap.rearrange("p (h two) k -> p h (two k)", two=2)
    wk2_ap = wk.rearrange("p (h two) k -> p h (two k)", two=2)
    tv = ar.alloc((16, 16), F32)
    b_tv = [Buf(f"tv{i}") for i in range(16)]
    ti = ar.alloc((16, 16), U32)
    b_ti = [Buf(f"ti{i}") for i in range(16)]
    tif = TB(ar.alloc((16, 16), F32), "tif")
    bv = ar.alloc((8, 16), F32)
    b_bv = [Buf(f"bv{i}") for i in range(8)]
    bp = ar.alloc((8, 16), U32)
    b_bp = [Buf(f"bp{i}") for i in range(8)]
    bpa = TB(ar.alloc((8, 16), U32), "bpa")
    bpb = TB(ar.alloc((8, 16), U32), "bpb")
    abf = TB(ar.alloc((2, 8, 16), F32), "abf")
    ge = TB(ar.alloc((8, 16), F32), "ge")
    gs = TB(ar.alloc((16,), F32), "gs")
    oh = TB(ar.alloc((8, 16, 16), F32), "oh")
    idx3 = Ring([TB(ar.alloc((3, 128), F32), f"idx3{i}") for i in range(2)])
    TS = 8
    Jr = Ring([TB(ar.alloc((128 * TS,), BF16), f"J{i}") for i in range(3)])
    W0r = Ring([TB(ar.alloc((64 * TS,), BF16), f"W0{i}") for i in range(2)])
    Wr = Ring([TB(ar.alloc((64 * TS,), BF16), f"W{i}") for i in range(3)])
    iota_jt = TB(ar.alloc((128 * TS,), BF16), "iota_jt")
    ucr = Ring([TB(ar.alloc((1024,), BF16), f"uc{i}") for i in range(4)])
    vcr = Ring([TB(ar.alloc((1024,), BF16), f"vc{i}") for i in range(6)])
    gar = Ring([TB(ar.alloc((TP,), BF16), f"ga{i}") for i in range(4)])
    GAr = Ring([TB(ar.alloc((TP,), BF16), f"GA{i}") for i in range(4)])
    pslabs = Ring([TB(ar.alloc((1024,), BF16), f"pslab{i}") for i in range(4)])
    print("PEER arena top bytes", ar.top * 4)
    acc = PS[0:4]
    cbanks = Ring([PS[4], PS[5]])
    abanks = Ring([PS[6], PS[7]])
    cp("dve", iota_jt.ap.rearrange("p (j t) -> p j t", t=TS), iotab.ap.unsqueeze(2).to_broadcast([128, 128, TS]), [iotab.b], [iota_jt.b])
    io_jt = iota_jt.ap.rearrange("p (j t) -> p j t", t=TS)
    io16 = iota16.unsqueeze(1).unsqueeze(1).to_broadcast([128, 8, 16, 16])
    tv4 = tv.rearrange("p (h two) k -> p h two k", two=2)
    tif4 = tif.ap.rearrange("p (h two) k -> p h two k", two=2)
    NP = NTOK // TP
    if stop_after == "P1":
        NP = 1

    def dvop(fn, r, w):
        S.op("dve", fn, r, w)

    def stage_A(p):
        r0 = p * TP
        x1t, h2T, trT = x1ts[p % 2], h2Ts[p % 2], trTs[p % 2]
        xn_, st_ = [xnr.next(), xnr.next()], [pst.next(), pst.next()]
        sls = {}
        for tb in range(2):
            rows = slice(r0 + tb * 128, r0 + (tb + 1) * 128)
            dma("sp", x1t[tb].ap, X1[rows, :], [b_X1[(r0 + tb * 128) // 128]], [x1t[tb].b])
        for hp in range(2):
            sls[hp] = pslabs.next()
            dma("sp", sls[hp].ap, Wpq_s[hp], [b_Wpq], [sls[hp].b])
        yield
        for tb in range(2):
            act(xn_[tb].ap, x1t[tb].ap, AF.Square, [x1t[tb].b], [xn_[tb].b, st_[tb].b], accum=st_[tb].ap[:, 0:1])
            act(st_[tb].ap[:, 1:2], st_[tb].ap[:, 0:1], AF.Sqrt, [st_[tb].b, smalls.b], [st_[tb].b], bias=eps_t, scale=1.0 / DM)
        yield
        for tb in range(2):
            recip(st_[tb].ap[:, 2:3], st_[tb].ap[:, 1:2], [st_[tb].b], [st_[tb].b])
            stt(xn_[tb].ap, x1t[tb].ap, st_[tb].ap[:, 2:3], gffn.ap, ALU.mult, ALU.mult, [x1t[tb].b, st_[tb].b, gffn.b], [xn_[tb].b])
        yield
        pbs_ = []
        for tb in range(2):
            pb = pbr.next()
            for dc in range(8):
                tr(pb.ap[:, dc * 128:(dc + 1) * 128], xn_[tb].ap[:, dc * 128:(dc + 1) * 128], identb.ap, [xn_[tb].b, identb.b], [pb.b])
            cp("act", h2T.ap[:, :, tb * 128:(tb + 1) * 128], pb.ap.rearrange("p (a b) -> p a b", a=8), [pb.b], [h2T.b])
            yield
        prev_bank = None
        for hp in range(16 + 1):
            if hp + 2 < 16:
                sls[hp + 2] = pslabs.next()
                dma("sp", sls[hp + 2].ap, Wpq_s[hp + 2], [b_Wpq], [sls[hp + 2].b])
            if hp < 16:
                sl = sls.pop(hp)
                bank = abanks.next()
                for kc in range(8):
                    mm(bank.ap[:, 0:TP], sl.ap[:, kc * 128:(kc + 1) * 128], h2T.ap[:, kc, :], kc == 0, kc == 7, [sl.b, h2T.b], [bank.b])
                cp("act", qpT.ap[:, hp, :], bank.ap[:, 0:TP], [bank.b], [qpT.b])
            yield
        for tb in range(2):
            id3 = idx3.next()
            for g4 in range(4):
                bank = abanks.next()
                for q in range(4):
                    hp = g4 * 4 + q
                    mm(bank.ap[:, q * 128:(q + 1) * 128], qpT.ap[:, hp, tb * 128:(tb + 1) * 128], skT.ap[:, hp, :], True, True,
                       [qpT.b, skT.b], [bank.b])
                cp("act" if g4 % 2 else "dve", sc.ap[:, g4 * 4:(g4 + 1) * 4, :], bank.ap.rearrange("p (a b) -> p a b", a=4), [bank.b], [sc.b])
            yield
            for hp in range(16):
                dvop(lambda e, hp=hp: e.max(out=tv[:, hp, 0:8], in_=sc.ap[:, hp, :]), [sc.b], [b_tv[hp]])
            yield
            for hp in range(16):
                dvop(lambda e, hp=hp: e.max_index(out=ti[:, hp, 0:8], in_max=tv[:, hp, 0:8], in_values=sc.ap[:, hp, :]), [sc.b, b_tv[hp]], [b_ti[hp]])
            yield
            for hp in range(16):
                dvop(lambda e, hp=hp: e.match_replace(out=wk[:, hp, :], in_to_replace=tv[:, hp, 0:8], in_values=sc.ap[:, hp, :], imm_value=-1e30),
                     [sc.b, b_tv[hp]], [b_wk[hp]])
            yield
            for hp in range(16):
                dvop(lambda e, hp=hp: e.max(out=tv[:, hp, 8:16], in_=wk[:, hp, :]), [b_wk[hp]], [b_tv[hp]])
            yield
            for hp in range(16):
                dvop(lambda e, hp=hp: e.max_index(out=ti[:, hp, 8:16], in_max=tv[:, hp, 8:16], in_values=wk[:, hp, :]), [b_wk[hp], b_tv[hp]], [b_ti[hp]])
            yield
            cp("dve", tif.ap, ti, b_ti, [tif.b])
            tt("dve", cand_ap.rearrange("p h (a b) -> p h a b", a=16), tv4[:, :, 0, :].unsqueeze(3).to_broadcast([128, 8, 16, 16]),
               tv4[:, :, 1, :].unsqueeze(2).to_broadcast([128, 8, 16, 16]), ALU.add, b_tv + [sc.b], [sc.b])
            yield
            for h in range(8):
                dvop(lambda e, h=h: e.max(out=bv[:, h, 0:8], in_=cand_ap[:, h, :]), [sc.b], [b_bv[h]])
            for h in range(8):
                dvop(lambda e, h=h: e.max_index(out=bp[:, h, 0:8], in_max=bv[:, h, 0:8], in_values=cand_ap[:, h, :]), [sc.b, b_bv[h]], [b_bp[h]])
            yield
            for h in range(8):
                dvop(lambda e, h=h: e.match_replace(out=wk2_ap[:, h, :], in_to_replace=bv[:, h, 0:8], in_values=cand_ap[:, h, :], imm_value=-1e30),
                     [sc.b, b_bv[h]] + b_wk, [b_wk[2 * h], b_wk[2 * h + 1]])
            yield
            for h in range(8):
                dvop(lambda e, h=h: e.max(out=bv[:, h, 8:16], in_=wk2_ap[:, h, :]), [b_wk[2 * h], b_wk[2 * h + 1]], [b_bv[h]])
            for h in range(8):
                dvop(lambda e, h=h: e.max_index(out=bp[:, h, 8:16], in_max=bv[:, h, 8:16], in_values=wk2_ap[:, h, :]),
                     [b_wk[2 * h], b_wk[2 * h + 1], b_bv[h]], [b_bp[h]])
            yield
            tt("dve", ge.ap, bv, bv[:, :, 0:1].to_broadcast([128, 8, 16]), ALU.subtract, b_bv, [ge.b])
            act(ge.ap, ge.ap, AF.Exp, [ge.b], [ge.b])
            red(gs.ap[:, 0:8], ge.ap, ALU.add, [ge.b], [gs.b])
            recip(gs.ap[:, 8:16], gs.ap[:, 0:8], [gs.b], [gs.b])
            tt("dve", id3.ap[:, 2, :].rearrange("p (h k) -> p h k", h=8), ge.ap, gs.ap[:, 8:16].unsqueeze(2).to_broadcast([128, 8, 16]), ALU.mult,
               [ge.b, gs.b], [id3.b])
            yield
            dvop(lambda e: e.tensor_single_scalar(out=bpa.ap, in_=bp, scalar=4, op=ALU.logical_shift_right), b_bp, [bpa.b])
            dvop(lambda e: e.tensor_single_scalar(out=bpb.ap, in_=bp, scalar=15, op=ALU.bitwise_and), b_bp, [bpb.b])
            cp("dve", abf.ap[:, 0, :, :], bpa.ap, [bpa.b], [abf.b])
            cp("dve", abf.ap[:, 1, :, :], bpb.ap, [bpb.b], [abf.b])
            yield
            for half in range(2):
                tt("dve", oh.ap, abf.ap[:, half, :, :].unsqueeze(3).to_broadcast([128, 8, 16, 16]), io16, ALU.is_equal, [abf.b, consts.b], [oh.b])
                tt("dve", oh.ap, oh.ap, tif4[:, :, half, :].unsqueeze(2).to_broadcast([128, 8, 16, 16]), ALU.mult, [oh.b, tif.b], [oh.b])
                red(id3.ap[:, half, :].rearrange("p (h k) -> p h k", h=8), oh.ap, ALU.add, [oh.b], [id3.b])
                yield
            yield
            for q in range(3):
                bank = abanks.next()
                tr(bank.ap[:, 0:128], id3.ap[:, q, :], identf, [id3.b, consts.b], [bank.b])
                cp("act" if q % 2 else "dve", trT.ap[:, q, tb * 128:(tb + 1) * 128], bank.ap[:, 0:128], [bank.b], [trT.b])
            yield

    b_GT = [Buf("GT_lo"), Buf("GT_hi")]

    def stage_B(p, h):
        trT = trTs[p % 2]
        i0 = h * 64
        NSB = TP // TS
        ctx = {}
        for k in range(NSB + 1):
            if k < NSB:
                ts0 = k * TS
                Jt, W0, Wt = Jr.next(), W0r.next(), Wr.next()
                J3 = Jt.ap.rearrange("p (t j) -> p t j", t=TS)
                W03 = W0.ap.rearrange("p (j t) -> p j t", t=TS)
                W3 = Wt.ap.rearrange("p (j t) -> p j t", t=TS)
                tt("dve", J3, iotab.ap.unsqueeze(1).to_broadcast([128, TS, 128]), trT.ap[:, 1, ts0:ts0 + TS].unsqueeze(2).to_broadcast([128, TS, 128]),
                   ALU.is_equal, [iotab.b, trT.b], [Jt.b])
                tt("dve", W03, io_jt[:, i0:i0 + 64, :], trT.ap[:, 0, ts0:ts0 + TS].unsqueeze(1).to_broadcast([128, 64, TS]), ALU.is_equal,
                   [iota_jt.b, trT.b], [W0.b])
                tt("dve", W3, W03, trT.ap[:, 2, ts0:ts0 + TS].unsqueeze(1).to_broadcast([128, 64, TS]), ALU.mult, [W0.b, trT.b], [Wt.b])
                ctx[k] = [Jt, Wt, J3, W3]
            k1 = k - 1
            if 0 <= k1 < NSB:
                Jt, Wt, J3, W3 = ctx[k1]
                bank = abanks.next()
                for tl in range(TS):
                    mm(bank.ap[:, tl * 64:(tl + 1) * 64], J3[:, tl, :], W3[:, :, tl], True, True, [Jt.b, Wt.b], [bank.b])
                ts0 = k1 * TS
                cp("act", GT.ap[:, i0:i0 + 64, ts0:ts0 + TS], bank.ap.rearrange("p (t i) -> p i t", t=TS), [bank.b], [b_GT[h]])
                del ctx[k1]
            yield

    def stage_C(p, sched):
        r0 = p * TP
        x1t, h2T = x1ts[p % 2], h2Ts[p % 2]

        def emit_AT(i):
            uc, vc = ucr.next(), vcr.next()
            dma("sp", uc.ap, UT_s[i * 128:(i + 1) * 128, :], [b_UT[i // 4]], [uc.b])
            dma("sp", vc.ap, V_s[i * 128:(i + 1) * 128, :], [b_V[i // 4]], [vc.b])
            ab = cbanks.next()
            for dc in range(8):
                mm(ab.ap[:, 0:TP], uc.ap[:, dc * 128:(dc + 1) * 128], h2T.ap[:, dc, :], dc == 0, dc == 7, [uc.b, h2T.b], [ab.b])
            return ab, vc

        def emit_gelu(i, ab):
            ga = gar.next()
            act(ga.ap, ab.ap[:, 0:TP], AF.Gelu_apprx_tanh, [ab.b], [ga.b])
            return ga

        def emit_mult(i, ga):
            GA = GAr.next()
            tt("dve", GA.ap, ga.ap, GT.ap[:, i, :], ALU.mult, [ga.b, b_GT[i // 64]], [GA.b])
            return GA

        def emit_out(i, GA, vc):
            for tb in range(2):
                for half in range(2):
                    a_ = acc[tb * 2 + half]
                    mm(a_.ap, GA.ap[:, tb * 128:(tb + 1) * 128], vc.ap[:, half * 512:(half + 1) * 512], i == 0, i == 127, [GA.b, vc.b], [a_.b])

        st_ab, st_vc, st_ga, st_GA = {}, {}, {}, {}
        st_ab[0], st_vc[0] = emit_AT(0)
        for i in range(128 + 2):
            if i + 1 < 128:
                st_ab[i + 1], st_vc[i + 1] = emit_AT(i + 1)
            if i < 128:
                st_ga[i] = emit_gelu(i, st_ab.pop(i))
            if 0 <= i - 1 < 128:
                st_GA[i - 1] = emit_mult(i - 1, st_ga.pop(i - 1))
            if 0 <= i - 2 < 128:
                emit_out(i - 2, st_GA.pop(i - 2), st_vc.pop(i - 2))
            if i < len(sched):
                next(sched[i], None)
        for g_ in sched[128 + 2:] + sched[:0]:
            next(g_, None)
        for g_ in dict.fromkeys(sched):
            for _ in g_:
                pass
        for tb in range(2):
            xt_ = x1t[tb]
            for half in range(2):
                a_ = acc[tb * 2 + half]
                hs = slice(half * 512, (half + 1) * 512)
                tt("dve", xt_.ap[:, hs], xt_.ap[:, hs], a_.ap, ALU.add, [xt_.b, a_.b], [xt_.b])
            jk, st = xnr.next(), pst.next()
            act(jk.ap, xt_.ap, AF.Square, [xt_.b], [jk.b, st.b], accum=st.ap[:, 0:1])
            act(st.ap[:, 1:2], st.ap[:, 0:1], AF.Sqrt, [st.b, smalls.b], [st.b], bias=eps_t, scale=1.0 / DM)
            recip(st.ap[:, 2:3], st.ap[:, 1:2], [st.b], [st.b])
            stt(xt_.ap, xt_.ap, st.ap[:, 2:3], gfin.ap, ALU.mult, ALU.mult, [xt_.b, st.b, gfin.b], [xt_.b])
            dma("sp", out_d[r0 + tb * 128:r0 + (tb + 1) * 128, :], xt_.ap, [xt_.b], [])

    import itertools
    for _ in stage_A(0):
        pass
    for _ in stage_B(0, 0):
        pass
    for p in range(NP):
        gBh = stage_B(p, 1)
        if p + 1 < NP:
            gA, gBl = stage_A(p + 1), stage_B(p + 1, 0)
            sched = []
            for _ in range(33):
                sched += [gBh, gA]
            sched += [gA] * 5
            for _ in range(16):
                sched += [gBl, gA]
            sched += [gBl] * 17
            assert sched.index(gBl) >= 66
        else:
            sched = [gBh] * 33
        stage_C(p, sched)
    S.finish()
    S.replay()
    print("arena peak bytes", ar.peak * 4)
    return nc


def _layouts(inp):
    f = lambda a: np.ascontiguousarray(np.asarray(a, dtype=np.float32))
    w_in = f(inp["w_in"])[0]
    slab = lambda w, nk: np.ascontiguousarray(
        w.reshape(nk, 128, w.shape[1] // 128, 128).transpose(2, 1, 0, 3).reshape(w.shape[1] // 128, 128, nk * 128))
    d = {}
    d["w_in_h"] = slab(w_in, 8)
    d["w_mkv_h"] = slab(f(inp["w_mem_kv"])[0], 8)
    d["w_pq_h"] = slab(f(inp["w_peer_q"])[0], 8)
    d["p_attn_h"] = slab(f(inp["p_attn"])[0], 6)
    d["p_lru_h"] = slab(f(inp["p_lru"])[0], 6)
    d["p_mem_h"] = slab(f(inp["p_mem"])[0], 4)
    d["w_out_h"] = np.ascontiguousarray(f(inp["w_out"])[0].reshape(8, 128, 1024).transpose(1, 0, 2).reshape(128, 8192))
    sk = f(inp["peer_sub_keys"])[0]
    d["skT_h"] = np.ascontiguousarray(sk.reshape(16, 128, 128).transpose(2, 0, 1).reshape(128, 2048))
    wr = f(inp["lru_wr"])[0].reshape(12, 128, 128)
    wi = f(inp["lru_wi"])[0].reshape(12, 128, 128)
    d["lruw_h"] = np.ascontiguousarray(np.concatenate([wr, wi], 0).transpose(1, 0, 2).reshape(128, 3072))
    u = f(inp["peer_u"])[0]
    d["uT_h"] = np.ascontiguousarray(u.reshape(128, 128, 8, 128).transpose(0, 3, 2, 1).reshape(128 * 128, 1024))
    d["v_h"] = f(inp["peer_v"])[0]
    vec = np.zeros((128, 96), np.float32)
    vec[:, 0:24] = f(inp["conv_w"])[0].reshape(4, 6, 128).transpose(2, 1, 0).reshape(128, 24)
    vec[:, 24:30] = f(inp["conv_b"])[0].reshape(6, 128).T
    vec[:, 30:42] = f(inp["lru_br"])[0].reshape(12, 128).T
    vec[:, 42:54] = f(inp["lru_bi"])[0].reshape(12, 128).T
    vec[:, 54:66] = f(inp["lru_lambda"])[0].reshape(12, 128).T
    vec[:, 66:90] = f(inp["gate_b"])[0].reshape(24, 128).T
    inv_freq = (np.float32(500000.0) ** (-np.arange(0, 32, 2, dtype=np.float32) / np.float32(32))).astype(np.float32)
    vec[0:32, 90] = np.concatenate([inv_freq, inv_freq])
    d["vec_h"] = vec
    bc = np.stack([f(inp["g_mix"])[0], f(inp["g_mem"])[0], f(inp["g_ffn"])[0], f(inp["g_final"])], 0)
    d["bc_h"] = np.ascontiguousarray(np.broadcast_to(bc[None], (128, 4, 1024)))
    d["sink_h"] = np.ascontiguousarray(np.broadcast_to(f(inp["attn_sink"])[0][None], (128, 6)))
    c = np.zeros((128, 816), np.float32)
    c[:, 0:128] = np.eye(128, dtype=np.float32)
    for m in range(16):
        c[m + 16, 688 + m] = -1.0
        c[m, 688 + 16 + m] = 1.0
    p = np.arange(128)[:, None]
    j = np.arange(384)[None, :]
    c[:, 160:544] = np.where((j >= p) & (j <= p + 256), 0.0, -1e30)
    c[:, 544:672] = np.arange(128, dtype=np.float32)[None, :]
    c[:, 672:688] = np.arange(16, dtype=np.float32)[None, :]
    d["consts_h"] = c
    return d


_NC_CACHE = {}


def kernel(**inputs):
    shared = _layouts(inputs)
    x = np.ascontiguousarray(np.asarray(inputs["x"], dtype=np.float32))
    mem = np.ascontiguousarray(np.asarray(inputs["mem"], dtype=np.float32))
    pos = np.ascontiguousarray(np.asarray(inputs["positions"]).astype(np.int32))
    in_maps = []
    for c in range(N_CORES):
        m = dict(shared)
        sl = slice(c * NSEQ_CORE, (c + 1) * NSEQ_CORE)
        m["x"] = x[sl]
        m["mem"] = mem[sl]
        m["pos"] = np.ascontiguousarray(np.broadcast_to(pos[sl][:, None, :], (NSEQ_CORE, 128, SEQ)))
        in_maps.append(m)
    nc = build_program()
    res = run_bass_kernel_spmd(nc, in_maps, core_ids=list(range(N_CORES)))
    outs = [np.asarray(r["out"]).reshape(NSEQ_CORE, SEQ, DM) for r in res.results]
    return np.concatenate(outs, 0).astype(np.float32)
```

```python
import numpy as np
import concourse.bass as bass
import concourse.mybir as mybir
from concourse.bass_utils import run_bass_kernel_spmd

F32 = mybir.dt.float32
BF16 = mybir.dt.bfloat16
I32 = mybir.dt.int32
U32 = mybir.dt.uint32
AF = mybir.ActivationFunctionType
ALU = mybir.AluOpType
AX = mybir.AxisListType

N_CORES = 8
SEQ = 2048
DM = 1024
NSEQ_CORE = 2
OQ, OKK, OV, OXL, OGL, OQM, OG = 0, 768, 1024, 1280, 2048, 2816, 3328
EPS = 1e-6
TP = 256


class Buf:
    __slots__ = ("name", "w", "r", "excl")

    def __init__(self, name="", excl=False):
        self.name = name
        self.w = None
        self.r = []
        self.excl = excl


class Ins:
    __slots__ = ("eng", "seq", "fn", "signal", "cnt", "dma", "sem", "target", "waits")


class Sched:
    ENGS = ["pe", "act", "dve", "pool", "sp"]

    def __init__(self, nc, n_dma_sems=16, n_pool_sems=3):
        self.nc = nc
        self.prog = {e: [] for e in self.ENGS}
        self.known = {e: {} for e in self.ENGS}
        self.sem = {e: nc.alloc_semaphore("s_" + e) for e in self.ENGS}
        nq = {"sp": n_dma_sems, "pool": n_pool_sems}
        self.dsem = {q: [nc.alloc_semaphore(f"d_{q}{i}") for i in range(nq[q])] for q in ("sp", "pool")}
        self.dcount = {q: [0] * nq[q] for q in ("sp", "pool")}
        self.dlast = {q: [None] * nq[q] for q in ("sp", "pool")}
        self.drr = {"sp": 0, "pool": 0}

    def _need(self, eng, dep, waits):
        if dep is None:
            return
        if dep.dma:
            key = ("d", id(dep.sem))
            if self.known[eng].get(key, 0) >= dep.target:
                return
            self.known[eng][key] = dep.target
            waits.append(dep)
        else:
            if dep.eng == "pe" and eng == "pe":
                return
            key = dep.eng
            if self.known[eng].get(key, -1) >= dep.seq:
                return
            self.known[eng][key] = dep.seq
            dep.signal = True
            waits.append(dep)

    def _deps(self, eng, reads, writes):
        cands = []
        for b in reads:
            if b.w is not None:
                cands.append(b.w)
            if b.excl:
                cands.extend(r for r in b.r if r.eng != eng)
        for b in writes:
            if b.w is not None:
                cands.append(b.w)
            cands.extend(b.r)
        best = {}
        for d in cands:
            key = ("d", id(d.sem)) if d.dma else d.eng
            val = d.target if d.dma else d.seq
            if key not in best or val > best[key][0]:
                best[key] = (val, d)
        waits = []
        for key in best:
            self._need(eng, best[key][1], waits)
        return waits

    def _commit(self, ins, reads, writes):
        for b in reads:
            b.r.append(ins)
        for b in writes:
            b.w = ins
            b.r = []

    def _new(self, eng, fn):
        ins = Ins()
        ins.eng = eng
        ins.fn = fn
        ins.dma = False
        ins.signal = False
        ins.cnt = None
        ins.waits = []
        return ins

    def op(self, eng, fn, reads=(), writes=()):
        ins = self._new(eng, fn)
        ins.waits = self._deps(eng, reads, writes)
        ins.seq = len(self.prog[eng])
        self.prog[eng].append(ins)
        self._commit(ins, reads, writes)
        return ins

    def dma(self, q, fn, reads=(), writes=()):
        ins = self._new(q, fn)
        ins.dma = True
        ins.signal = True
        k = self.drr[q]
        self.drr[q] = (k + 1) % len(self.dsem[q])
        ins.sem = self.dsem[q][k]
        self.dcount[q][k] += 1
        ins.target = 16 * self.dcount[q][k]
        ins.waits = self._deps(q, reads, writes)
        prev = self.dlast[q][k]
        if prev is not None:
            self._need(q, prev, ins.waits)
        self.dlast[q][k] = ins
        ins.seq = len(self.prog[q])
        self.prog[q].append(ins)
        self._commit(ins, reads, writes)
        return ins

    def _lasts(self):
        lasts = []
        for e in self.ENGS:
            for i in reversed(self.prog[e]):
                if (not i.dma) and i.fn is not None:
                    lasts.append(i)
                    break
        for q in ("sp", "pool"):
            for i in self.dlast[q]:
                if i is not None:
                    lasts.append(i)
        return lasts

    def barrier(self, engines=None):
        lasts = self._lasts()
        for e in engines or self.ENGS:
            waits = []
            for d in lasts:
                if (not d.dma) and d.eng == e:
                    continue
                self._need(e, d, waits)
            if waits:
                ins = self._new(e, None)
                ins.waits = waits
                ins.seq = len(self.prog[e])
                self.prog[e].append(ins)

    def finish(self):
        self.barrier(engines=["sp"])

    def replay(self):
        nc = self.nc
        for e in self.ENGS:
            c = 0
            for i in self.prog[e]:
                if i.dma or i.fn is None:
                    continue
                if i.signal:
                    c += 1
                    i.cnt = c
        with nc.Block() as block:
            def run(engname, handle):
                for i in self.prog[engname]:
                    for d in i.waits:
                        if d.dma:
                            handle.wait_ge(d.sem, d.target)
                        else:
                            handle.wait_ge(self.sem[d.eng], d.cnt)
                    if i.fn is None:
                        continue
                    r = i.fn(handle)
                    if i.dma:
                        r.then_inc(i.sem, 16)
                    elif i.signal:
                        r.then_inc(self.sem[engname], 1)

            @block.tensor
            def _(t):
                run("pe", t)

            @block.scalar
            def _(s):
                run("act", s)

            @block.vector
            def _(v):
                run("dve", v)

            @block.gpsimd
            def _(g):
                run("pool", g)

            @block.sync
            def _(s):
                run("sp", s)


class Arena:
    ESZ = {F32: 4, BF16: 2, I32: 4, U32: 4}

    def __init__(self, ap, nwords):
        self.ap = ap
        self.n = nwords
        self.top = 0
        self.peak = 0

    def alloc(self, free_shape, dt, parts=128):
        n = int(np.prod(free_shape))
        nw = (n * self.ESZ[dt] + 3) // 4
        nw8 = (nw + 7) // 8 * 8
        o = self.top
        self.top += nw8
        self.peak = max(self.peak, self.top)
        assert self.top <= self.n, f"arena overflow {self.top*4} > {self.n*4}"
        v = self.ap[0:parts, o:o + nw]
        if dt != F32:
            v = v.bitcast(dt)
        if len(free_shape) == 2:
            v = v.rearrange("p (a b) -> p a b", a=free_shape[0])
        elif len(free_shape) == 3:
            v = v.rearrange("p (a b c) -> p a b c", a=free_shape[0], b=free_shape[1])
        return v

    def mark(self):
        return self.top

    def release(self, m):
        self.top = m


class Ring:
    def __init__(self, items):
        self.items = items
        self.i = 0

    def next(self):
        it = self.items[self.i]
        self.i = (self.i + 1) % len(self.items)
        return it


class TB:
    __slots__ = ("ap", "b")

    def __init__(self, ap, name="", excl=False):
        self.ap = ap
        self.b = Buf(name, excl)


def build_program(NSEQ=NSEQ_CORE, stop_after=None):
    nc = bass.Bass("TRN2", target_bir_lowering=False)
    NTOK = NSEQ * SEQ

    def din(name, shape, dt=F32):
        return nc.dram_tensor(name, list(shape), dt, kind="ExternalInput").ap()

    def dscr(name, shape, dt):
        return nc.dram_tensor(name, list(shape), dt).ap()

    x_d = din("x", [NSEQ, SEQ, DM])
    mem_d = din("mem", [NSEQ, 256, DM])
    pos_d = din("pos", [NSEQ, 128, SEQ], I32)
    win_h = din("w_in_h", [50, 128, 1024])
    wmkv_h = din("w_mkv_h", [8, 128, 1024])
    wpq_h = din("w_pq_h", [16, 128, 1024])
    pattn_h = din("p_attn_h", [8, 128, 768])
    plru_h = din("p_lru_h", [8, 128, 768])
    pmem_h = din("p_mem_h", [8, 128, 512])
    wout_h = din("w_out_h", [128, 8192])
    skT_h = din("skT_h", [128, 2048])
    lruw_h = din("lruw_h", [128, 3072])
    uT_h = din("uT_h", [128 * 128, 1024])
    v_h = din("v_h", [128 * 128, 1024])
    vec_h = din("vec_h", [128, 96])
    bc_h = din("bc_h", [128, 4, 1024])
    sink_h = din("sink_h", [128, 6])
    consts_h = din("consts_h", [128, 816])
    if stop_after in (None, "P1"):
        out_d = nc.dram_tensor("out", [NTOK, DM], F32, kind="ExternalOutput").ap()
        X1 = dscr("X1", [NTOK, DM], F32)
    else:
        X1 = nc.dram_tensor("out", [NTOK, DM], F32, kind="ExternalOutput").ap()
        out_d = None

    Win_s = dscr("Win_s", [50, 128, 1024], BF16)
    Wmkv_s = dscr("Wmkv_s", [8, 128, 1024], BF16)
    Wpq_s = dscr("Wpq_s", [16, 128, 1024], BF16)
    Pattn_s = dscr("Pattn_s", [8, 128, 768], BF16)
    Plru_s = dscr("Plru_s", [8, 128, 768], BF16)
    Pmem_s = dscr("Pmem_s", [8, 128, 512], BF16)
    UT_s = dscr("UT_s", [128 * 128, 1024], BF16)
    V_s = dscr("V_s", [128 * 128, 1024], BF16)

    S = Sched(nc)
    b_X1 = [Buf(f"X1_{i}") for i in range(NTOK // 128)]
    AW = 52992
    arena_t = nc.alloc_sbuf_tensor("arena", [128, AW], F32)
    ar = Arena(arena_t[:], AW)
    PS = [TB(nc.alloc_psum_tensor(f"ps{i}", [128, 512], F32)[:], f"ps{i}", True) for i in range(8)]
    PF = PS[0:6]
    PB = []
    for i in (6, 7):
        t_ = TB(PS[i].ap.bitcast(BF16), f"pb{i}")
        t_.b = PS[i].b
        PB.append(t_)
    pbr = Ring(PB)
    pfr = Ring(PF)

    def mm(out, lhsT, rhs, start, stop, r, w):
        S.op("pe", lambda e: e.matmul(out, lhsT=lhsT, rhs=rhs, start=start, stop=stop), r, w)

    def tr(out, in_, ident, r, w):
        S.op("pe", lambda e: e.transpose(out=out, in_=in_, identity=ident), r, w)

    def act(out, in_, func, r, w, bias=None, scale=1.0, accum=None):
        kw = {}
        if bias is not None:
            kw["bias"] = bias
        if accum is not None:
            kw["accum_out"] = accum
        S.op("act", lambda e: e.activation(out=out, in_=in_, func=func, scale=scale, **kw), r, w)

    def cp(eng, out, in_, r, w):
        if eng == "act":
            S.op("act", lambda e: e.copy(out=out, in_=in_), r, w)
        else:
            S.op(eng, lambda e: e.tensor_copy(out=out, in_=in_), r, w)

    def tt(eng, out, in0, in1, op, r, w):
        S.op(eng, lambda e: e.tensor_tensor(out=out, in0=in0, in1=in1, op=op), r, w)

    def ts(eng, out, in0, s1, s2, op0, op1, r, w):
        if s2 is None:
            S.op(eng, lambda e: e.tensor_scalar(out=out, in0=in0, scalar1=s1, scalar2=None, op0=op0), r, w)
        else:
            S.op(eng, lambda e: e.tensor_scalar(out=out, in0=in0, scalar1=s1, scalar2=s2, op0=op0, op1=op1), r, w)

    def stt(out, in0, scalar, in1, op0, op1, r, w):
        S.op("dve", lambda e: e.scalar_tensor_tensor(out=out, in0=in0, scalar=scalar, in1=in1, op0=op0, op1=op1), r, w)

    def red(out, in_, op, r, w):
        S.op("dve", lambda e: e.tensor_reduce(out=out, in_=in_, axis=AX.X, op=op), r, w)

    def recip(out, in_, r, w):
        S.op("dve", lambda e: e.reciprocal(out=out, in_=in_), r, w)

    def dma(q, out, in_, r, w, **kw):
        S.dma(q, lambda e: e.dma_start(out=out, in_=in_, **kw), r, w)

    def memset(eng, ap, val, w):
        S.op(eng, lambda e: e.memset(ap, val), (), w)

    b_Win = [Buf() for _ in range(50)]
    b_Wmkv, b_Wpq, b_Pattn, b_Plru, b_Pmem = Buf(), Buf(), Buf(), Buf(), Buf()
    b_UT = [Buf() for _ in range(32)]
    b_V = [Buf() for _ in range(32)]
    CK = dict(max_dma_last_dim=4096)

    consts = TB(ar.alloc((816,), F32), "consts")
    dma("sp", consts.ap, consts_h, [], [consts.b])
    identf = consts.ap[:, 0:128]
    mask = consts.ap[:, 160:544]
    iota128 = consts.ap[:, 544:672]
    iota16 = consts.ap[:, 672:688]
    identb = TB(ar.alloc((128,), BF16), "identb")
    dma("pool", identb.ap, consts_h[:, 0:128], [], [identb.b])
    permb = TB(ar.alloc((128,), BF16), "permb")
    dma("pool", permb.ap, consts_h[:, 688:816], [], [permb.b])
    maskb = TB(ar.alloc((384,), BF16), "maskb")
    dma("pool", maskb.ap, consts_h[:, 160:544], [], [maskb.b])
    vec = TB(ar.alloc((96,), F32), "vec")
    dma("sp", vec.ap, vec_h, [], [vec.b])
    sinkb = TB(ar.alloc((8,), F32), "sinkb")
    dma("sp", sinkb.ap[:, 0:6], sink_h, [], [sinkb.b])
    smalls = TB(ar.alloc((8,), F32), "smalls")
    memset("pool", smalls.ap[:, 0:1], EPS, [smalls.b])
    memset("pool", smalls.ap[:, 1:2], 1.0, [smalls.b])
    eps_t = smalls.ap[:, 0:1]
    one_t = smalls.ap[:, 1:2]
    lamv = TB(ar.alloc((16,), F32), "lamv")
    CW, CB, BR, BI, LAM, GB, INVF = 0, 24, 30, 42, 54, 66, 90

    def conv_win(g):
        dma("pool", Win_s[g * 5:(g + 1) * 5].rearrange("a p f -> (a p) f"),
            win_h[g * 5:(g + 1) * 5].rearrange("a p f -> (a p) f"), [], [b_Win[g * 5 + k] for k in range(5)], **CK)

    dma("pool", Wmkv_s.rearrange("a p f -> (a p) f"), wmkv_h.rearrange("a p f -> (a p) f"), [], [b_Wmkv], **CK)
    for g in (2, 3, 4, 0, 1):
        conv_win(g)

    def late_conv():
        for g in (5, 6, 7, 8, 9):
            conv_win(g)
        late_conv_rest()

    def late_conv_rest():
        dma("pool", Pattn_s.rearrange("a p f -> (a p) f"), pattn_h.rearrange("a p f -> (a p) f"), [], [b_Pattn], **CK)
        dma("pool", Plru_s.rearrange("a p f -> (a p) f"), plru_h.rearrange("a p f -> (a p) f"), [], [b_Plru], **CK)
        dma("pool", Pmem_s.rearrange("a p f -> (a p) f"), pmem_h.rearrange("a p f -> (a p) f"), [], [b_Pmem], **CK)
        for g in range(2):
            dma("pool", Wpq_s[g * 8:(g + 1) * 8].rearrange("a p f -> (a p) f"),
                wpq_h[g * 8:(g + 1) * 8].rearrange("a p f -> (a p) f"), [], [b_Wpq], **CK)

    def table_conv():
        for g in range(32):
            dma("pool", UT_s[g * 512:(g + 1) * 512, :], uT_h[g * 512:(g + 1) * 512, :], [], [b_UT[g]], **CK)
            yield
            dma("pool", V_s[g * 512:(g + 1) * 512, :], v_h[g * 512:(g + 1) * 512, :], [], [b_V[g]], **CK)
            yield

    tconv = table_conv()

    def conv_some(n):
        if stop_after not in (None, "P1"):
            return
        for _ in range(n):
            try:
                next(tconv)
            except StopIteration:
                return

    mtmp = ar.mark()
    t_e = TB(ar.alloc((16,), F32)); t_u = TB(ar.alloc((16,), F32)); t_l = TB(ar.alloc((16,), F32))
    lam_ap = vec.ap[:, LAM:LAM + 12]
    act(t_e.ap[:, 0:12], lam_ap, AF.Exp, [vec.b], [t_e.b], scale=-1.0)
    ts("dve", t_u.ap[:, 0:12], t_e.ap[:, 0:12], 1.0, None, ALU.add, None, [t_e.b], [t_u.b])
    act(t_l.ap[:, 0:12], t_u.ap[:, 0:12], AF.Ln, [t_u.b], [t_l.b])
    ts("dve", t_u.ap[:, 0:12], t_u.ap[:, 0:12], -1.0, 1e-30, ALU.add, ALU.max, [t_u.b], [t_u.b])
    recip(t_u.ap[:, 0:12], t_u.ap[:, 0:12], [t_u.b], [t_u.b])
    tt("dve", t_l.ap[:, 0:12], t_l.ap[:, 0:12], t_u.ap[:, 0:12], ALU.mult, [t_l.b, t_u.b], [t_l.b])
    tt("dve", t_l.ap[:, 0:12], t_l.ap[:, 0:12], t_e.ap[:, 0:12], ALU.mult, [t_l.b, t_e.b], [t_l.b])
    ts("dve", lamv.ap[:, 0:12], t_l.ap[:, 0:12], -8.0, None, ALU.mult, None, [t_l.b], [lamv.b])

    SCALE = float(128 ** -0.5)
    m_phase = ar.mark()

    def early_exit():
        S.finish()
        S.replay()
        return nc

    if stop_after == "P0":
        return early_exit()

    hT = ar.alloc((8, SEQ), BF16)
    b_hT = [Buf(f"hT{i}") for i in range(4)]
    kT = TB(ar.alloc((2, SEQ), BF16), "kT")
    vtok = TB(ar.alloc((16, 256), BF16), "vtok")
    lruT = ar.alloc((6, SEQ), BF16)
    b_lruT = [Buf(f"lruT{i}") for i in range(6)]
    Ct = TB(ar.alloc((SEQ,), F32), "C")
    St = TB(ar.alloc((SEQ,), F32), "S")
    kmT = TB(ar.alloc((4, 256), BF16), "kmT")
    vm = TB(ar.alloc((2, 512), BF16), "vm")
    wout = TB(ar.alloc((8, 1024), BF16), "wout")
    slabs = Ring([TB(ar.alloc((1024,), BF16), f"slab{i}") for i in range(8)])
    dma("pool", wout.ap.rearrange("p a b -> p (a b)"), wout_h, [], [wout.b], **CK)
    r_mark = ar.mark()

    def load_slab(src_ap, src_bufs, n=1024):
        sl = slabs.next()
        dma("sp", sl.ap[:, 0:n], src_ap, src_bufs, [sl.b])
        return sl

    def proj_fm(fc_src_ap, src_bufs, rhs_fn, rhs_bufs, ncols, nk=8):
        sl = load_slab(fc_src_ap, src_bufs, nk * 128)
        bank = pfr.next()
        for kc in range(nk):
            mm(bank.ap[:, 0:ncols], sl.ap[:, kc * 128:(kc + 1) * 128], rhs_fn(kc), kc == 0, kc == nk - 1,
               [sl.b] + rhs_bufs, [bank.b])
        return bank

    def rmsnorm_T(src_rows_ap, gb_ap, gb_buf, xt, xn, stat, dstT_ap, dst_bufs, cp_eng, src_bufs=()):
        dma("sp", xt.ap, src_rows_ap, list(src_bufs), [xt.b])
        act(xn.ap, xt.ap, AF.Square, [xt.b], [xn.b, stat.b], accum=stat.ap[:, 0:1])
        act(stat.ap[:, 1:2], stat.ap[:, 0:1], AF.Sqrt, [stat.b, smalls.b], [stat.b], bias=eps_t, scale=1.0 / DM)
        recip(stat.ap[:, 2:3], stat.ap[:, 1:2], [stat.b], [stat.b])
        stt(xn.ap, xt.ap, stat.ap[:, 2:3], gb_ap, ALU.mult, ALU.mult, [xt.b, stat.b, gb_buf], [xn.b])
        pb = pbr.next()
        for dc in range(8):
            tr(pb.ap[:, dc * 128:(dc + 1) * 128], xn.ap[:, dc * 128:(dc + 1) * 128], identb.ap, [xn.b, identb.b], [pb.b])
        cp(cp_eng, dstT_ap, pb.ap.rearrange("p (a b) -> p a b", a=8), [pb.b], dst_bufs)

    def range_reduce_sin(out_tb, ang_tb, tmpf, tmpi, n, parts=128):
        TWO_PI = float(2 * np.pi)
        a = ang_tb.ap[0:parts, 0:n]
        kf = tmpf.ap[0:parts, 0:n]
        ki = tmpi.ap[0:parts, 0:n]
        ts("dve", kf, a, 1.0 / TWO_PI, None, ALU.mult, None, [ang_tb.b], [tmpf.b])
        cp("dve", ki, kf, [tmpf.b], [tmpi.b])
        cp("dve", kf, ki, [tmpi.b], [tmpf.b])
        stt(a, kf, -TWO_PI, a, ALU.mult, ALU.add, [tmpf.b, ang_tb.b], [ang_tb.b])
        ts("dve", kf, a, float(np.pi), -TWO_PI, ALU.is_gt, ALU.mult, [ang_tb.b], [tmpf.b])
        tt("dve", a, a, kf, ALU.add, [ang_tb.b, tmpf.b], [ang_tb.b])
        ts("dve", kf, a, float(-np.pi), TWO_PI, ALU.is_lt, ALU.mult, [ang_tb.b], [tmpf.b])
        tt("dve", a, a, kf, ALU.add, [ang_tb.b, tmpf.b], [ang_tb.b])
        act(out_tb.ap[0:parts, 0:n], a, AF.Sin, [ang_tb.b], [out_tb.b])

    def rope_evac(bank, dst_ap, dst_bufs, tok0, n, tmpb, tmpa, tmpc):
        cp("act", tmpb.ap[:, 0:n], bank.ap[:, 0:n], [bank.b], [tmpb.b])
        pbk = pfr.next()
        mm(pbk.ap[:, 0:n], permb.ap, tmpb.ap[:, 0:n], True, True, [permb.b, tmpb.b], [pbk.b])
        tt("dve", tmpa.ap[:, 0:n], bank.ap[:, 0:n], Ct.ap[:, tok0:tok0 + n], ALU.mult, [bank.b, Ct.b], [tmpa.b])
        tt("dve", tmpc.ap[:, 0:n], pbk.ap[:, 0:n], St.ap[:, tok0:tok0 + n], ALU.mult, [pbk.b, St.b], [tmpc.b])
        tt("dve", dst_ap, tmpa.ap[:, 0:n], tmpc.ap[:, 0:n], ALU.add, [tmpa.b, tmpc.b], dst_bufs)

    for s in range(NSEQ):
        if s > 0:
            S.barrier()
        ar.release(r_mark)
        gmix = TB(ar.alloc((1024,), F32), "gmix")
        gmem = TB(ar.alloc((1024,), F32), "gmem")
        dma("sp", gmix.ap, bc_h[:, 0, :], [], [gmix.b])
        dma("sp", gmem.ap, bc_h[:, 1, :], [], [gmem.b])
        xts = [TB(ar.alloc((1024,), F32), f"xt{i}") for i in range(4)]
        xns = [TB(ar.alloc((1024,), BF16), f"xn{i}") for i in range(4)]
        stats = [TB(ar.alloc((8,), F32), f"st{i}") for i in range(4)]
        memT = TB(ar.alloc((8, 256), BF16), "memT")
        posi = TB(ar.alloc((SEQ,), I32), "posi")
        ang = TB(ar.alloc((SEQ,), F32), "ang")
        ang2 = TB(ar.alloc((SEQ,), F32), "ang2")
        tmpf = TB(ar.alloc((SEQ,), F32), "tmpf")
        tmpi = TB(ar.alloc((SEQ,), I32), "tmpi")
        for tb in range(16):
            k = tb % 4
            rmsnorm_T(x_d[s, tb * 128:(tb + 1) * 128, :], gmix.ap, gmix.b, xts[k], xns[k], stats[k],
                      hT[:, :, tb * 128:(tb + 1) * 128], [b_hT[tb // 4]], "act" if k else "dve")
            if stop_after == "M0a":
                return early_exit()
        if stop_after == "M0b":
            return early_exit()
        for mb in range(2):
            rmsnorm_T(mem_d[s, mb * 128:(mb + 1) * 128, :], gmem.ap, gmem.b, xts[mb], xns[mb], stats[mb],
                      memT.ap[:, :, mb * 128:(mb + 1) * 128], [memT.b], "act" if mb else "dve")
        dma("sp", posi.ap, pos_d[s], [], [posi.b])
        cp("dve", ang.ap, posi.ap, [posi.b], [ang.b])
        ts("dve", ang.ap, ang.ap, vec.ap[:, INVF:INVF + 1], None, ALU.mult, None, [ang.b, vec.b], [ang.b])
        ts("dve", ang2.ap, ang.ap, float(np.pi / 2), None, ALU.add, None, [ang.b], [ang2.b])
        range_reduce_sin(St, ang, tmpf, tmpi, SEQ)
        range_reduce_sin(Ct, ang2, tmpf, tmpi, SEQ)
        if stop_after == "M0c":
            return early_exit()
        for hd in range(4):
            bank = proj_fm(Wmkv_s[hd], [b_Wmkv], lambda kc: memT.ap[:, kc, :], [memT.b], 256)
            cp("act", kmT.ap[:, hd, :], bank.ap[:, 0:256], [bank.b], [kmT.b])
            if stop_after == "M0d":
                return early_exit()
        if stop_after == "M0e":
            return early_exit()
        for hd in range(4):
            sl = load_slab(Wmkv_s[4 + hd], [b_Wmkv])
            for mb in range(2):
                bank = pfr.next()
                for kc in range(8):
                    mm(bank.ap[:, 0:128], memT.ap[:, kc, mb * 128:(mb + 1) * 128], sl.ap[:, kc * 128:(kc + 1) * 128],
                       kc == 0, kc == 7, [memT.b, sl.b], [bank.b])
                cp("dve", vm.ap[:, mb, hd * 128:(hd + 1) * 128], bank.ap[:, 0:128], [bank.b], [vm.b])
        conv_some(4 if s else 0)
        if stop_after == "M0":
            return early_exit()

        S.barrier()
        ar.release(r_mark)
        lruw = TB(ar.alloc((24, 128), BF16), "lruw")
        dma("pool", lruw.ap.rearrange("p a b -> p (a b)"), lruw_h, [], [lruw.b], **CK)
        if s == 0:
            late_conv()
        T = [TB(ar.alloc((SEQ + 8,), F32), f"T{i}") for i in range(6)]
        xcb = TB(ar.alloc((SEQ,), BF16), "xcb")
        for cb in range(6):
            xl, xc, t2, t3, t4, t5 = T
            memset("pool", xl.ap[:, 0:2], 0.0, [xl.b])
            memset("pool", xl.ap[:, SEQ + 2:SEQ + 3], 0.0, [xl.b])
            sl = load_slab(Win_s[OXL // 128 + cb], [b_Win[OXL // 128 + cb]])
            for tc in range(4):
                bank = pfr.next()
                for kc in range(8):
                    mm(bank.ap, sl.ap[:, kc * 128:(kc + 1) * 128], hT[:, kc, tc * 512:(tc + 1) * 512], kc == 0, kc == 7,
                       [sl.b, b_hT[tc]], [bank.b])
                cp("act" if tc % 2 else "dve", xl.ap[:, 2 + tc * 512:2 + (tc + 1) * 512], bank.ap, [bank.b], [xl.b])
            cw = lambda j: vec.ap[:, CW + cb * 4 + j:CW + cb * 4 + j + 1]
            ts("dve", xc.ap[:, 0:SEQ], xl.ap[:, 0:SEQ], cw(0), vec.ap[:, CB + cb:CB + cb + 1], ALU.mult, ALU.add,
               [xl.b, vec.b], [xc.b])
            for j in range(1, 4):
                stt(xc.ap[:, 0:SEQ], xl.ap[:, j:j + SEQ], cw(j), xc.ap[:, 0:SEQ], ALU.mult, ALU.add, [xl.b, vec.b, xc.b], [xc.b])
            cp("act", xcb.ap, xc.ap[:, 0:SEQ], [xc.b], [xcb.b])
            for d in range(2):
                col = d * 6 + cb
                for (which, dst, bo) in ((0, t2, BR), (1, t3, BI)):
                    for tc in range(4):
                        bank = pfr.next()
                        mm(bank.ap, lruw.ap[:, which * 12 + col, :], xcb.ap[:, tc * 512:(tc + 1) * 512], True, True,
                           [lruw.b, xcb.b], [bank.b])
                        act(dst.ap[:, tc * 512:(tc + 1) * 512], bank.ap, AF.Sigmoid, [bank.b, vec.b], [dst.b],
                            bias=vec.ap[:, bo + col:bo + col + 1])
                a_ap = t2.ap[:, 0:SEQ]
                act(a_ap, a_ap, AF.Exp, [t2.b, lamv.b], [t2.b], scale=lamv.ap[:, col:col + 1])
                stt(t4.ap[:, 0:SEQ], a_ap, -1.0, a_ap, ALU.mult, ALU.mult, [t2.b], [t4.b])
                act(t4.ap[:, 0:SEQ], t4.ap[:, 0:SEQ], AF.Sqrt, [t4.b, smalls.b], [t4.b], bias=one_t)
                tt("dve", t3.ap[:, 0:SEQ], t3.ap[:, 0:SEQ], t4.ap[:, 0:SEQ], ALU.mult, [t3.b, t4.b], [t3.b])
                tt("dve", t3.ap[:, 0:SEQ], t3.ap[:, 0:SEQ], xc.ap[:, 0:SEQ], ALU.mult, [t3.b, xc.b], [t3.b])
                if d == 0:
                    S.op("dve", lambda e, o=t5.ap[:, 0:SEQ], a0=a_ap, b0=t3.ap[:, 0:SEQ]: e.tensor_tensor_scan(
                        out=o, data0=a0, data1=b0, initial=0.0, op0=ALU.mult, op1=ALU.add), [t2.b, t3.b], [t5.b])
                else:
                    S.op("dve", lambda e, o=t4.ap[:, SEQ - 1::-1], a0=t2.ap[:, SEQ - 1::-1], b0=t3.ap[:, SEQ - 1::-1]:
                         e.tensor_tensor_scan(out=o, data0=a0, data1=b0, initial=0.0, op0=ALU.mult, op1=ALU.add),
                         [t2.b, t3.b], [t4.b])
            tt("dve", t5.ap[:, 0:SEQ], t5.ap[:, 0:SEQ], t4.ap[:, 0:SEQ], ALU.add, [t5.b, t4.b], [t5.b])
            sl = load_slab(Win_s[OGL // 128 + cb], [b_Win[OGL // 128 + cb]])
            for tc in range(4):
                bank = pfr.next()
                for kc in range(8):
                    mm(bank.ap, sl.ap[:, kc * 128:(kc + 1) * 128], hT[:, kc, tc * 512:(tc + 1) * 512], kc == 0, kc == 7,
                       [sl.b, b_hT[tc]], [bank.b])
                act(xl.ap[:, tc * 512:(tc + 1) * 512], bank.ap, AF.Gelu_apprx_tanh, [bank.b], [xl.b])
            tt("dve", lruT[:, cb, :], xl.ap[:, 0:SEQ], t5.ap[:, 0:SEQ], ALU.mult, [xl.b, t5.b], [b_lruT[cb]])
            conv_some(2)

        if stop_after == "M1":
            return early_exit()
        S.barrier()
        ar.release(r_mark)
        qT = TB(ar.alloc((6, 512), BF16), "qT")
        qmT = TB(ar.alloc((4, 512), BF16), "qmT")
        aoT = TB(ar.alloc((6, 512), BF16), "aoT")
        moT = TB(ar.alloc((4, 512), BF16), "moT")
        mgT = TB(ar.alloc((8, 512), BF16), "mgT")
        gts = Ring([TB(ar.alloc((512,), F32), f"g{i}") for i in range(2)])
        accm = TB(ar.alloc((512,), F32), "accm")
        tmpm = Ring([TB(ar.alloc((512,), F32), f"tmpm{i}") for i in range(1)])
        xres = Ring([TB(ar.alloc((1024,), F32), f"xres{i}") for i in range(4)])
        rtb = Ring([TB(ar.alloc((512,), BF16), f"rtb{i}") for i in range(2)])
        rta = Ring([TB(ar.alloc((512,), F32), f"rta{i}") for i in range(1)])
        rtc = Ring([TB(ar.alloc((512,), F32), f"rtc{i}") for i in range(1)])
        NH = 6
        s_sb = [TB(ar.alloc((384,), F32), f"s{i}") for i in range(NH)]
        Pn = [TB(ar.alloc((384,), BF16), f"Pn{i}") for i in range(NH)]
        PT = [TB(ar.alloc((384,), BF16), f"PT{i}") for i in range(NH)]
        sm = [TB(ar.alloc((8,), F32), f"sm{i}") for i in range(NH)]

        for kvh in range(2):
            for tc in range(4):
                bank = proj_fm(Win_s[OKK // 128 + kvh], [b_Win[OKK // 128 + kvh]],
                               lambda kc: hT[:, kc, tc * 512:(tc + 1) * 512], [b_hT[tc]], 512)
                if stop_after == "M2p":
                    return early_exit()
                rope_evac(bank, kT.ap[:, kvh, tc * 512:(tc + 1) * 512], [kT.b], tc * 512, 512, rtb.next(), rta.next(), rtc.next())
                if stop_after == "M2a":
                    return early_exit()
        if stop_after == "M2b":
            return early_exit()
        for kvh in range(2):
            sl = load_slab(Win_s[OV // 128 + kvh], [b_Win[OV // 128 + kvh]])
            for tb in range(16):
                bank = pfr.next()
                for kc in range(8):
                    mm(bank.ap[:, 0:128], hT[:, kc, tb * 128:(tb + 1) * 128], sl.ap[:, kc * 128:(kc + 1) * 128],
                       kc == 0, kc == 7, [b_hT[tb // 4], sl.b], [bank.b])
                cp("act" if tb % 2 else "dve", vtok.ap[:, tb, kvh * 128:(kvh + 1) * 128], bank.ap[:, 0:128], [bank.b], [vtok.b])
        conv_some(4)
        if stop_after == "M2":
            return early_exit()

        def softmax_block(nh, score_fn, NK, mask_ap, sink_col, v_fn, nkb, out_fn):
            banks = []
            for i in range(nh):
                bank = pfr.next()
                score_fn(i, bank, mask_ap is None)
                if mask_ap is not None:
                    mm(bank.ap[:, 0:NK], identb.ap, mask_ap, False, True, [identb.b, maskb.b], [bank.b])
                banks.append(bank)
            for i in range(nh):
                red(sm[i].ap[:, 0:1], banks[i].ap[:, 0:NK], ALU.max, [banks[i].b], [sm[i].b])
                if sink_col is not None:
                    ts("dve", sm[i].ap[:, 0:1], sm[i].ap[:, 0:1], SCALE, sinkb.ap[:, sink_col(i):sink_col(i) + 1], ALU.mult, ALU.max,
                       [sm[i].b, sinkb.b], [sm[i].b])
                    ts("dve", sm[i].ap[:, 1:2], sm[i].ap[:, 0:1], -1.0, None, ALU.mult, None, [sm[i].b], [sm[i].b])
                else:
                    ts("dve", sm[i].ap[:, 1:2], sm[i].ap[:, 0:1], -SCALE, None, ALU.mult, None, [sm[i].b], [sm[i].b])
            for i in range(nh):
                sa = s_sb[i].ap[:, 0:NK]
                act(sa, banks[i].ap[:, 0:NK], AF.Exp, [banks[i].b, sm[i].b], [s_sb[i].b, sm[i].b], bias=sm[i].ap[:, 1:2], scale=SCALE,
                    accum=sm[i].ap[:, 2:3])
                if sink_col is not None:
                    act(sm[i].ap[:, 3:4], sm[i].ap[:, 1:2], AF.Exp, [sm[i].b, sinkb.b], [sm[i].b],
                        bias=sinkb.ap[:, sink_col(i):sink_col(i) + 1])
            for i in range(nh):
                sa = s_sb[i].ap[:, 0:NK]
                if sink_col is not None:
                    tt("dve", sm[i].ap[:, 4:5], sm[i].ap[:, 2:3], sm[i].ap[:, 3:4], ALU.add, [sm[i].b], [sm[i].b])
                    recip(sm[i].ap[:, 5:6], sm[i].ap[:, 4:5], [sm[i].b], [sm[i].b])
                else:
                    recip(sm[i].ap[:, 5:6], sm[i].ap[:, 2:3], [sm[i].b], [sm[i].b])
                ts("dve", Pn[i].ap[:, 0:NK], sa, sm[i].ap[:, 5:6], None, ALU.mult, None, [s_sb[i].b, sm[i].b], [Pn[i].b])
            pbs = []
            for i in range(nh):
                pb = pbr.next()
                for j in range(nkb):
                    tr(pb.ap[:, j * 128:(j + 1) * 128], Pn[i].ap[:, j * 128:(j + 1) * 128], identb.ap, [Pn[i].b, identb.b], [pb.b])
                cp("act", PT[i].ap[:, 0:NK], pb.ap[:, 0:NK], [pb.b], [PT[i].b])
            for i in range(nh):
                ob = pfr.next()
                for j in range(nkb):
                    vap, vb = v_fn(i, j)
                    mm(ob.ap[:, 0:128], vap, PT[i].ap[:, j * 128:(j + 1) * 128], j == 0, j == nkb - 1, vb + [PT[i].b], [ob.b])
                dst, db = out_fn(i)
                cp("act", dst, ob.ap[:, 0:128], [ob.b], db)

        for tc in range(4):
            t0 = tc * 512
            hb = [b_hT[tc]]
            rhs_h = lambda kc: hT[:, kc, t0:t0 + 512]
            for h in range(6):
                bank = proj_fm(Win_s[OQ // 128 + h], [b_Win[OQ // 128 + h]], rhs_h, hb, 512)
                rope_evac(bank, qT.ap[:, h, :], [qT.b], t0, 512, rtb.next(), rta.next(), rtc.next())
            for hd in range(4):
                bank = proj_fm(Win_s[OQM // 128 + hd], [b_Win[OQM // 128 + hd]], rhs_h, hb, 512)
                cp("act", qmT.ap[:, hd, :], bank.ap, [bank.b], [qmT.b])
            for qb in range(4):
                n = tc * 4 + qb
                kb0, kb1 = max(0, n - 1), min(15, n + 1)
                nkb = kb1 - kb0 + 1
                NK = nkb * 128
                moff = 0 if n > 0 else 128
                qs = slice(qb * 128, (qb + 1) * 128)

                def score_a(i, bank, stop, qs=qs, kb0=kb0, NK=NK):
                    mm(bank.ap[:, 0:NK], qT.ap[:, i, qs], kT.ap[:, i // 3, kb0 * 128:kb0 * 128 + NK], True, stop,
                       [qT.b, kT.b], [bank.b])

                softmax_block(6, score_a, NK, maskb.ap[:, moff:moff + NK], lambda i: i,
                              lambda i, j, kb0=kb0: (vtok.ap[:, kb0 + j, (i // 3) * 128:(i // 3 + 1) * 128], [vtok.b]),
                              nkb, lambda i, qs=qs: (aoT.ap[:, i, qs], [aoT.b]))

                def score_m(i, bank, stop, qs=qs):
                    mm(bank.ap[:, 0:256], qmT.ap[:, i, qs], kmT.ap[:, i, :], True, stop, [qmT.b, kmT.b], [bank.b])

                softmax_block(4, score_m, 256, None, None,
                              lambda i, j: (vm.ap[:, j, i * 128:(i + 1) * 128], [vm.b]),
                              2, lambda i, qs=qs: (moT.ap[:, i, qs], [moT.b]))
            xrs = []
            for tb in range(4):
                xr = xres.next()
                dma("sp", xr.ap, x_d[s, t0 + tb * 128:t0 + (tb + 1) * 128, :], [], [xr.b])
                xrs.append(xr)
            for dmc in range(8):
                for b in range(3):
                    fc = OG // 128 + b * 8 + dmc
                    gbank = proj_fm(Win_s[fc], [b_Win[fc]], rhs_h, hb, 512)
                    gt = gts.next()
                    act(gt.ap, gbank.ap, AF.Sigmoid, [gbank.b, vec.b], [gt.b], bias=vec.ap[:, GB + b * 8 + dmc:GB + b * 8 + dmc + 1])
                    if b == 0:
                        pbank = proj_fm(Pattn_s[dmc], [b_Pattn], lambda kc: aoT.ap[:, kc, :], [aoT.b], 512, nk=6)
                    elif b == 1:
                        pbank = proj_fm(Plru_s[dmc], [b_Plru], lambda kc: lruT[:, kc, t0:t0 + 512], [b_lruT[kc2] for kc2 in range(6)], 512, nk=6)
                    else:
                        pbank = proj_fm(Pmem_s[dmc], [b_Pmem], lambda kc: moT.ap[:, kc, :], [moT.b], 512, nk=4)
                    if b == 0:
                        tt("dve", accm.ap, gt.ap, pbank.ap, ALU.mult, [gt.b, pbank.b], [accm.b])
                    elif b == 1:
                        tm = tmpm.next()
                        tt("dve", tm.ap, gt.ap, pbank.ap, ALU.mult, [gt.b, pbank.b], [tm.b])
                        tt("pool", accm.ap, accm.ap, tm.ap, ALU.add, [accm.b, tm.b], [accm.b])
                    else:
                        tm = tmpm.next()
                        tt("dve", tm.ap, gt.ap, pbank.ap, ALU.mult, [gt.b, pbank.b], [tm.b])
                        tt("pool", mgT.ap[:, dmc, :], accm.ap, tm.ap, ALU.add, [accm.b, tm.b], [mgT.b])
            for tb in range(4):
                row0 = s * SEQ + t0 + tb * 128
                xr = xrs[tb]
                for half in range(2):
                    bank = pfr.next()
                    for dmc in range(8):
                        mm(bank.ap, mgT.ap[:, dmc, tb * 128:(tb + 1) * 128], wout.ap[:, dmc, half * 512:(half + 1) * 512],
                           dmc == 0, dmc == 7, [mgT.b, wout.b], [bank.b])
                    tt("dve", xr.ap[:, half * 512:(half + 1) * 512], xr.ap[:, half * 512:(half + 1) * 512], bank.ap, ALU.add,
                       [xr.b, bank.b], [xr.b])
                dma("sp", X1[row0:row0 + 128, :], xr.ap, [xr.b], [b_X1[row0 // 128]])
            conv_some(6)

    if stop_after == "M":
        S.finish()
        S.replay()
        return nc

    conv_some(1000)
    S.barrier()
    ar.release(m_phase)
    skT = TB(ar.alloc((16, 128), BF16), "skT")
    dma("pool", skT.ap.rearrange("p a b -> p (a b)"), skT_h, [], [skT.b], **CK)
    gffn = TB(ar.alloc((1024,), F32), "gffn")
    gfin = TB(ar.alloc((1024,), F32), "gfin")
    dma("sp", gffn.ap, bc_h[:, 2, :], [], [gffn.b])
    dma("sp", gfin.ap, bc_h[:, 3, :], [], [gfin.b])
    GT = TB(ar.alloc((128, TP), BF16), "GT")
    h2Ts = [TB(ar.alloc((8, TP), BF16), f"h2T{i}") for i in range(2)]
    x1ts = [[TB(ar.alloc((1024,), F32), f"x1t{k}{i}") for i in range(2)] for k in range(2)]
    trTs = [TB(ar.alloc((3, TP), BF16), f"trT{i}") for i in range(2)]
    iotab = TB(ar.alloc((128,), BF16), "iotab")
    dma("pool", iotab.ap, consts_h[:, 544:672], [], [iotab.b])
    xnr = Ring([TB(ar.alloc((1024,), BF16), f"xnp{i}") for i in range(2)])
    pst = Ring([TB(ar.alloc((8,), F32), f"pst{i}") for i in range(4)])
    qpT = TB(ar.alloc((16, TP), BF16), "qpT")
    sc = TB(ar.alloc((16, 128), F32), "sc")
    wk = ar.alloc((16, 128), F32)
    b_wk = [Buf(f"wk{i}") for i in range(16)]
    cand_ap = sc.ap.rearrange("p (h two) k -> p h (two k)", two=2)
    wk2_ap = wk.rearrange("p (h two) k -> p h (two k)", two=2)
    tv = ar.alloc((16, 16), F32)
    b_tv = [Buf(f"tv{i}") for i in range(16)]
    ti = ar.alloc((16, 16), U32)
    b_ti = [Buf(f"ti{i}") for i in range(16)]
    tif = TB(ar.alloc((16, 16), F32), "tif")
    bv = ar.alloc((8, 16), F32)
    b_bv = [Buf(f"bv{i}") for i in range(8)]
    bp = ar.alloc((8, 16), U32)
    b_bp = [Buf(f"bp{i}") for i in range(8)]
    bpa = TB(ar.alloc((8, 16), U32), "bpa")
    bpb = TB(ar.alloc((8, 16), U32), "bpb")
    abf = TB(ar.alloc((2, 8, 16), F32), "abf")
    ge = TB(ar.alloc((8, 16), F32), "ge")
    gs = TB(ar.alloc((16,), F32), "gs")
    oh = TB(ar.alloc((8, 16, 16), F32), "oh")
    idx3 = Ring([TB(ar.alloc((3, 128), F32), f"idx3{i}") for i in range(2)])
    TS = 8
    Jr = Ring([TB(ar.alloc((128 * TS,), BF16), f"J{i}") for i in range(3)])
    W0r = Ring([TB(ar.alloc((64 * TS,), BF16), f"W0{i}") for i in range(2)])
    Wr = Ring([TB(ar.alloc((64 * TS,), BF16), f"W{i}") for i in range(3)])
    iota_jt = TB(ar.alloc((128 * TS,), BF16), "iota_jt")
    ucr = Ring([TB(ar.alloc((1024,), BF16), f"uc{i}") for i in range(4)])
    vcr = Ring([TB(ar.alloc((1024,), BF16), f"vc{i}") for i in range(6)])
    gar = Ring([TB(ar.alloc((TP,), BF16), f"ga{i}") for i in range(4)])
    GAr = Ring([TB(ar.alloc((TP,), BF16), f"GA{i}") for i in range(4)])
    pslabs = Ring([TB(ar.alloc((1024,), BF16), f"pslab{i}") for i in range(4)])
    acc = PS[0:4]
    cbanks = Ring([PS[4], PS[5]])
    abanks = Ring([PS[6], PS[7]])
    cp("dve", iota_jt.ap.rearrange("p (j t) -> p j t", t=TS), iotab.ap.unsqueeze(2).to_broadcast([128, 128, TS]), [iotab.b], [iota_jt.b])
    io_jt = iota_jt.ap.rearrange("p (j t) -> p j t", t=TS)
    io16 = iota16.unsqueeze(1).unsqueeze(1).to_broadcast([128, 8, 16, 16])
    tv4 = tv.rearrange("p (h two) k -> p h two k", two=2)
    tif4 = tif.ap.rearrange("p (h two) k -> p h two k", two=2)
    NP = NTOK // TP
    if stop_after == "P1":
        NP = 1

    def dvop(fn, r, w):
        S.op("dve", fn, r, w)

    def stage_A(p):
        r0 = p * TP
        x1t, h2T, trT = x1ts[p % 2], h2Ts[p % 2], trTs[p % 2]
        xn_, st_ = [xnr.next(), xnr.next()], [pst.next(), pst.next()]
        sls = {}
        for tb in range(2):
            rows = slice(r0 + tb * 128, r0 + (tb + 1) * 128)
            dma("sp", x1t[tb].ap, X1[rows, :], [b_X1[(r0 + tb * 128) // 128]], [x1t[tb].b])
        for hp in range(2):
            sls[hp] = pslabs.next()
            dma("sp", sls[hp].ap, Wpq_s[hp], [b_Wpq], [sls[hp].b])
        yield
        for tb in range(2):
            act(xn_[tb].ap, x1t[tb].ap, AF.Square, [x1t[tb].b], [xn_[tb].b, st_[tb].b], accum=st_[tb].ap[:, 0:1])
            act(st_[tb].ap[:, 1:2], st_[tb].ap[:, 0:1], AF.Sqrt, [st_[tb].b, smalls.b], [st_[tb].b], bias=eps_t, scale=1.0 / DM)
        yield
        for tb in range(2):
            recip(st_[tb].ap[:, 2:3], st_[tb].ap[:, 1:2], [st_[tb].b], [st_[tb].b])
            stt(xn_[tb].ap, x1t[tb].ap, st_[tb].ap[:, 2:3], gffn.ap, ALU.mult, ALU.mult, [x1t[tb].b, st_[tb].b, gffn.b], [xn_[tb].b])
        yield
        pbs_ = []
        for tb in range(2):
            pb = pbr.next()
            for dc in range(8):
                tr(pb.ap[:, dc * 128:(dc + 1) * 128], xn_[tb].ap[:, dc * 128:(dc + 1) * 128], identb.ap, [xn_[tb].b, identb.b], [pb.b])
            cp("act", h2T.ap[:, :, tb * 128:(tb + 1) * 128], pb.ap.rearrange("p (a b) -> p a b", a=8), [pb.b], [h2T.b])
            yield
        prev_bank = None
        for hp in range(16 + 1):
            if hp + 2 < 16:
                sls[hp + 2] = pslabs.next()
                dma("sp", sls[hp + 2].ap, Wpq_s[hp + 2], [b_Wpq], [sls[hp + 2].b])
            if hp < 16:
                sl = sls.pop(hp)
                bank = abanks.next()
                for kc in range(8):
                    mm(bank.ap[:, 0:TP], sl.ap[:, kc * 128:(kc + 1) * 128], h2T.ap[:, kc, :], kc == 0, kc == 7, [sl.b, h2T.b], [bank.b])
                cp("act", qpT.ap[:, hp, :], bank.ap[:, 0:TP], [bank.b], [qpT.b])
            yield
        for tb in range(2):
            id3 = idx3.next()
            for g4 in range(4):
                bank = abanks.next()
                for q in range(4):
                    hp = g4 * 4 + q
                    mm(bank.ap[:, q * 128:(q + 1) * 128], qpT.ap[:, hp, tb * 128:(tb + 1) * 128], skT.ap[:, hp, :], True, True,
                       [qpT.b, skT.b], [bank.b])
                cp("act" if g4 % 2 else "dve", sc.ap[:, g4 * 4:(g4 + 1) * 4, :], bank.ap.rearrange("p (a b) -> p a b", a=4), [bank.b], [sc.b])
            yield
            for hp in range(16):
                dvop(lambda e, hp=hp: e.max(out=tv[:, hp, 0:8], in_=sc.ap[:, hp, :]), [sc.b], [b_tv[hp]])
            yield
            for hp in range(16):
                dvop(lambda e, hp=hp: e.max_index(out=ti[:, hp, 0:8], in_max=tv[:, hp, 0:8], in_values=sc.ap[:, hp, :]), [sc.b, b_tv[hp]], [b_ti[hp]])
            yield
            for hp in range(16):
                dvop(lambda e, hp=hp: e.match_replace(out=wk[:, hp, :], in_to_replace=tv[:, hp, 0:8], in_values=sc.ap[:, hp, :], imm_value=-1e30),
                     [sc.b, b_tv[hp]], [b_wk[hp]])
            yield
            for hp in range(16):
                dvop(lambda e, hp=hp: e.max(out=tv[:, hp, 8:16], in_=wk[:, hp, :]), [b_wk[hp]], [b_tv[hp]])
            yield
            for hp in range(16):
                dvop(lambda e, hp=hp: e.max_index(out=ti[:, hp, 8:16], in_max=tv[:, hp, 8:16], in_values=wk[:, hp, :]), [b_wk[hp], b_tv[hp]], [b_ti[hp]])
            yield
            cp("dve", tif.ap, ti, b_ti, [tif.b])
            tt("dve", cand_ap.rearrange("p h (a b) -> p h a b", a=16), tv4[:, :, 0, :].unsqueeze(3).to_broadcast([128, 8, 16, 16]),
               tv4[:, :, 1, :].unsqueeze(2).to_broadcast([128, 8, 16, 16]), ALU.add, b_tv + [sc.b], [sc.b])
            yield
            for h in range(8):
                dvop(lambda e, h=h: e.max(out=bv[:, h, 0:8], in_=cand_ap[:, h, :]), [sc.b], [b_bv[h]])
            for h in range(8):
                dvop(lambda e, h=h: e.max_index(out=bp[:, h, 0:8], in_max=bv[:, h, 0:8], in_values=cand_ap[:, h, :]), [sc.b, b_bv[h]], [b_bp[h]])
            yield
            for h in range(8):
                dvop(lambda e, h=h: e.match_replace(out=wk2_ap[:, h, :], in_to_replace=bv[:, h, 0:8], in_values=cand_ap[:, h, :], imm_value=-1e30),
                     [sc.b, b_bv[h]] + b_wk, [b_wk[2 * h], b_wk[2 * h + 1]])
            yield
            for h in range(8):
                dvop(lambda e, h=h: e.max(out=bv[:, h, 8:16], in_=wk2_ap[:, h, :]), [b_wk[2 * h], b_wk[2 * h + 1]], [b_bv[h]])
            for h in range(8):
                dvop(lambda e, h=h: e.max_index(out=bp[:, h, 8:16], in_max=bv[:, h, 8:16], in_values=wk2_ap[:, h, :]),
                     [b_wk[2 * h], b_wk[2 * h + 1], b_bv[h]], [b_bp[h]])
            yield
            tt("dve", ge.ap, bv, bv[:, :, 0:1].to_broadcast([128, 8, 16]), ALU.subtract, b_bv, [ge.b])
            act(ge.ap, ge.ap, AF.Exp, [ge.b], [ge.b])
            red(gs.ap[:, 0:8], ge.ap, ALU.add, [ge.b], [gs.b])
            recip(gs.ap[:, 8:16], gs.ap[:, 0:8], [gs.b], [gs.b])
            tt("dve", id3.ap[:, 2, :].rearrange("p (h k) -> p h k", h=8), ge.ap, gs.ap[:, 8:16].unsqueeze(2).to_broadcast([128, 8, 16]), ALU.mult,
               [ge.b, gs.b], [id3.b])
            yield
            dvop(lambda e: e.tensor_single_scalar(out=bpa.ap, in_=bp, scalar=4, op=ALU.logical_shift_right), b_bp, [bpa.b])
            dvop(lambda e: e.tensor_single_scalar(out=bpb.ap, in_=bp, scalar=15, op=ALU.bitwise_and), b_bp, [bpb.b])
            cp("dve", abf.ap[:, 0, :, :], bpa.ap, [bpa.b], [abf.b])
            cp("dve", abf.ap[:, 1, :, :], bpb.ap, [bpb.b], [abf.b])
            yield
            for half in range(2):
                tt("dve", oh.ap, abf.ap[:, half, :, :].unsqueeze(3).to_broadcast([128, 8, 16, 16]), io16, ALU.is_equal, [abf.b, consts.b], [oh.b])
                tt("dve", oh.ap, oh.ap, tif4[:, :, half, :].unsqueeze(2).to_broadcast([128, 8, 16, 16]), ALU.mult, [oh.b, tif.b], [oh.b])
                red(id3.ap[:, half, :].rearrange("p (h k) -> p h k", h=8), oh.ap, ALU.add, [oh.b], [id3.b])
                yield
            yield
            for q in range(3):
                bank = abanks.next()
                tr(bank.ap[:, 0:128], id3.ap[:, q, :], identf, [id3.b, consts.b], [bank.b])
                cp("act" if q % 2 else "dve", trT.ap[:, q, tb * 128:(tb + 1) * 128], bank.ap[:, 0:128], [bank.b], [trT.b])
            yield

    b_GT = [Buf("GT_lo"), Buf("GT_hi")]

    def stage_B(p, h):
        trT = trTs[p % 2]
        i0 = h * 64
        NSB = TP // TS
        ctx = {}
        for k in range(NSB + 1):
            if k < NSB:
                ts0 = k * TS
                Jt, W0, Wt = Jr.next(), W0r.next(), Wr.next()
                J3 = Jt.ap.rearrange("p (t j) -> p t j", t=TS)
                W03 = W0.ap.rearrange("p (j t) -> p j t", t=TS)
                W3 = Wt.ap.rearrange("p (j t) -> p j t", t=TS)
                tt("dve", J3, iotab.ap.unsqueeze(1).to_broadcast([128, TS, 128]), trT.ap[:, 1, ts0:ts0 + TS].unsqueeze(2).to_broadcast([128, TS, 128]),
                   ALU.is_equal, [iotab.b, trT.b], [Jt.b])
                tt("dve", W03, io_jt[:, i0:i0 + 64, :], trT.ap[:, 0, ts0:ts0 + TS].unsqueeze(1).to_broadcast([128, 64, TS]), ALU.is_equal,
                   [iota_jt.b, trT.b], [W0.b])
                tt("dve", W3, W03, trT.ap[:, 2, ts0:ts0 + TS].unsqueeze(1).to_broadcast([128, 64, TS]), ALU.mult, [W0.b, trT.b], [Wt.b])
                ctx[k] = [Jt, Wt, J3, W3]
            k1 = k - 1
            if 0 <= k1 < NSB:
                Jt, Wt, J3, W3 = ctx[k1]
                bank = abanks.next()
                for tl in range(TS):
                    mm(bank.ap[:, tl * 64:(tl + 1) * 64], J3[:, tl, :], W3[:, :, tl], True, True, [Jt.b, Wt.b], [bank.b])
                ts0 = k1 * TS
                cp("act", GT.ap[:, i0:i0 + 64, ts0:ts0 + TS], bank.ap.rearrange("p (t i) -> p i t", t=TS), [bank.b], [b_GT[h]])
                del ctx[k1]
            yield

    def stage_C(p, sched):
        r0 = p * TP
        x1t, h2T = x1ts[p % 2], h2Ts[p % 2]

        def emit_AT(i):
            uc, vc = ucr.next(), vcr.next()
            dma("sp", uc.ap, UT_s[i * 128:(i + 1) * 128, :], [b_UT[i // 4]], [uc.b])
            dma("sp", vc.ap, V_s[i * 128:(i + 1) * 128, :], [b_V[i // 4]], [vc.b])
            ab = cbanks.next()
            for dc in range(8):
                mm(ab.ap[:, 0:TP], uc.ap[:, dc * 128:(dc + 1) * 128], h2T.ap[:, dc, :], dc == 0, dc == 7, [uc.b, h2T.b], [ab.b])
            return ab, vc

        def emit_gelu(i, ab):
            ga = gar.next()
            act(ga.ap, ab.ap[:, 0:TP], AF.Gelu_apprx_tanh, [ab.b], [ga.b])
            return ga

        def emit_mult(i, ga):
            GA = GAr.next()
            tt("dve", GA.ap, ga.ap, GT.ap[:, i, :], ALU.mult, [ga.b, b_GT[i // 64]], [GA.b])
            return GA

        def emit_out(i, GA, vc):
            for tb in range(2):
                for half in range(2):
                    a_ = acc[tb * 2 + half]
                    mm(a_.ap, GA.ap[:, tb * 128:(tb + 1) * 128], vc.ap[:, half * 512:(half + 1) * 512], i == 0, i == 127, [GA.b, vc.b], [a_.b])

        st_ab, st_vc, st_ga, st_GA = {}, {}, {}, {}
        st_ab[0], st_vc[0] = emit_AT(0)
        for i in range(128 + 2):
            if i + 1 < 128:
                st_ab[i + 1], st_vc[i + 1] = emit_AT(i + 1)
            if i < 128:
                st_ga[i] = emit_gelu(i, st_ab.pop(i))
            if 0 <= i - 1 < 128:
                st_GA[i - 1] = emit_mult(i - 1, st_ga.pop(i - 1))
            if 0 <= i - 2 < 128:
                emit_out(i - 2, st_GA.pop(i - 2), st_vc.pop(i - 2))
            if i < len(sched):
                next(sched[i], None)
        for g_ in sched[128 + 2:] + sched[:0]:
            next(g_, None)
        for g_ in dict.fromkeys(sched):
            for _ in g_:
                pass
        for tb in range(2):
            xt_ = x1t[tb]
            for half in range(2):
                a_ = acc[tb * 2 + half]
                hs = slice(half * 512, (half + 1) * 512)
                tt("dve", xt_.ap[:, hs], xt_.ap[:, hs], a_.ap, ALU.add, [xt_.b, a_.b], [xt_.b])
            jk, st = xnr.next(), pst.next()
            act(jk.ap, xt_.ap, AF.Square, [xt_.b], [jk.b, st.b], accum=st.ap[:, 0:1])
            act(st.ap[:, 1:2], st.ap[:, 0:1], AF.Sqrt, [st.b, smalls.b], [st.b], bias=eps_t, scale=1.0 / DM)
            recip(st.ap[:, 2:3], st.ap[:, 1:2], [st.b], [st.b])
            stt(xt_.ap, xt_.ap, st.ap[:, 2:3], gfin.ap, ALU.mult, ALU.mult, [xt_.b, st.b, gfin.b], [xt_.b])
            dma("sp", out_d[r0 + tb * 128:r0 + (tb + 1) * 128, :], xt_.ap, [xt_.b], [])

    import itertools
    for _ in stage_A(0):
        pass
    for _ in stage_B(0, 0):
        pass
    for p in range(NP):
        gBh = stage_B(p, 1)
        if p + 1 < NP:
            gA, gBl = stage_A(p + 1), stage_B(p + 1, 0)
            sched = []
            for _ in range(33):
                sched += [gBh, gA]
            sched += [gA] * 5
            for _ in range(16):
                sched += [gBl, gA]
            sched += [gBl] * 17
            assert sched.index(gBl) >= 66
        else:
            sched = [gBh] * 33
        stage_C(p, sched)
    S.finish()
    S.replay()
    return nc


def _layouts(inp):
    f = lambda a: np.ascontiguousarray(np.asarray(a, dtype=np.float32))
    w_in = f(inp["w_in"])[0]
    slab = lambda w, nk: np.ascontiguousarray(
        w.reshape(nk, 128, w.shape[1] // 128, 128).transpose(2, 1, 0, 3).reshape(w.shape[1] // 128, 128, nk * 128))
    d = {}
    d["w_in_h"] = slab(w_in, 8)
    d["w_mkv_h"] = slab(f(inp["w_mem_kv"])[0], 8)
    d["w_pq_h"] = slab(f(inp["w_peer_q"])[0], 8)
    d["p_attn_h"] = slab(f(inp["p_attn"])[0], 6)
    d["p_lru_h"] = slab(f(inp["p_lru"])[0], 6)
    d["p_mem_h"] = slab(f(inp["p_mem"])[0], 4)
    d["w_out_h"] = np.ascontiguousarray(f(inp["w_out"])[0].reshape(8, 128, 1024).transpose(1, 0, 2).reshape(128, 8192))
    sk = f(inp["peer_sub_keys"])[0]
    d["skT_h"] = np.ascontiguousarray(sk.reshape(16, 128, 128).transpose(2, 0, 1).reshape(128, 2048))
    wr = f(inp["lru_wr"])[0].reshape(12, 128, 128)
    wi = f(inp["lru_wi"])[0].reshape(12, 128, 128)
    d["lruw_h"] = np.ascontiguousarray(np.concatenate([wr, wi], 0).transpose(1, 0, 2).reshape(128, 3072))
    u = f(inp["peer_u"])[0]
    d["uT_h"] = np.ascontiguousarray(u.reshape(128, 128, 8, 128).transpose(0, 3, 2, 1).reshape(128 * 128, 1024))
    d["v_h"] = f(inp["peer_v"])[0]
    vec = np.zeros((128, 96), np.float32)
    vec[:, 0:24] = f(inp["conv_w"])[0].reshape(4, 6, 128).transpose(2, 1, 0).reshape(128, 24)
    vec[:, 24:30] = f(inp["conv_b"])[0].reshape(6, 128).T
    vec[:, 30:42] = f(inp["lru_br"])[0].reshape(12, 128).T
    vec[:, 42:54] = f(inp["lru_bi"])[0].reshape(12, 128).T
    vec[:, 54:66] = f(inp["lru_lambda"])[0].reshape(12, 128).T
    vec[:, 66:90] = f(inp["gate_b"])[0].reshape(24, 128).T
    inv_freq = (np.float32(500000.0) ** (-np.arange(0, 32, 2, dtype=np.float32) / np.float32(32))).astype(np.float32)
    vec[0:32, 90] = np.concatenate([inv_freq, inv_freq])
    d["vec_h"] = vec
    bc = np.stack([f(inp["g_mix"])[0], f(inp["g_mem"])[0], f(inp["g_ffn"])[0], f(inp["g_final"])], 0)
    d["bc_h"] = np.ascontiguousarray(np.broadcast_to(bc[None], (128, 4, 1024)))
    d["sink_h"] = np.ascontiguousarray(np.broadcast_to(f(inp["attn_sink"])[0][None], (128, 6)))
    c = np.zeros((128, 816), np.float32)
    c[:, 0:128] = np.eye(128, dtype=np.float32)
    for m in range(16):
        c[m + 16, 688 + m] = -1.0
        c[m, 688 + 16 + m] = 1.0
    p = np.arange(128)[:, None]
    j = np.arange(384)[None, :]
    c[:, 160:544] = np.where((j >= p) & (j <= p + 256), 0.0, -1e30)
    c[:, 544:672] = np.arange(128, dtype=np.float32)[None, :]
    c[:, 672:688] = np.arange(16, dtype=np.float32)[None, :]
    d["consts_h"] = c
    return d


_NC_CACHE = {}


def kernel(**inputs):
    shared = _layouts(inputs)
    x = np.ascontiguousarray(np.asarray(inputs["x"], dtype=np.float32))
    mem = np.ascontiguousarray(np.asarray(inputs["mem"], dtype=np.float32))
    pos = np.ascontiguousarray(np.asarray(inputs["positions"]).astype(np.int32))
    in_maps = []
    for c in range(N_CORES):
        m = dict(shared)
        sl = slice(c * NSEQ_CORE, (c + 1) * NSEQ_CORE)
        m["x"] = x[sl]
        m["mem"] = mem[sl]
        m["pos"] = np.ascontiguousarray(np.broadcast_to(pos[sl][:, None, :], (NSEQ_CORE, 128, SEQ)))
        in_maps.append(m)
    nc = build_program()
    res = run_bass_kernel_spmd(nc, in_maps, core_ids=list(range(N_CORES)))
    outs = [np.asarray(r["out"]).reshape(NSEQ_CORE, SEQ, DM) for r in res.results]
    return np.concatenate(outs, 0).astype(np.float32)
```
